# Optimizing a Trainium2 kernel written in Bass

```python
import jax, jax.numpy as jnp
from jax import lax
import numpy as np

D_MODEL = 2048
BATCH = 16
SEQ = 2048
DEPTH = 4

CHUNK = 64
N_MEM = 256
D_MIX = D_MODEL
POOL_W = D_MIX // 2
POOL_GROUPS = 4
POOL_GROUP_W = POOL_W // POOL_GROUPS
POOL_WINDOWS = (2, 4, 8, 16)
MLSTM_W = D_MIX - POOL_W
MLSTM_HEADS = 4
MLSTM_HEAD_DIM = MLSTM_W // MLSTM_HEADS
CONV_W = 4
D_IN = POOL_W + 4 * MLSTM_W + 2 * MLSTM_HEADS
XATTN_HEADS = 4
XATTN_HEAD_DIM = D_MODEL // XATTN_HEADS
D_FF = ((8 * D_MODEL // 3 + 255) // 256) * 256
EPS = 1e-6

kernel_name = "hymba_pool_mlstm_macaron_xattn"


def rms_norm(x, g):
    xf = x.astype(jnp.float32)
    y = xf * lax.rsqrt(jnp.mean(xf * xf, axis=-1, keepdims=True) + EPS)
    return (y * g.astype(jnp.float32)).astype(x.dtype)


def swiglu(x, w_gate, w_up, w_down):
    return (jax.nn.silu(x @ w_gate) * (x @ w_up)) @ w_down


def causal_dwconv(x, w):
    K = w.shape[0]
    S = x.shape[1]
    xp = jnp.pad(x, ((0, 0), (K - 1, 0), (0, 0)))
    y = xp[:, 0:S] * w[0]
    for j in range(1, K):
        y = y + xp[:, j:j + S] * w[j]
    return y


def multiscale_pool(p, pool_w, pool_scale):
    B, S, _ = p.shape
    pg = p.reshape(B, S, POOL_GROUPS, POOL_GROUP_W).astype(jnp.float32)
    cs = jnp.cumsum(pg, axis=1)
    t = jnp.arange(1, S + 1, dtype=jnp.float32)
    outs = []
    for g, w in enumerate(POOL_WINDOWS):
        c = cs[:, :, g]
        lag = jnp.pad(c[:, :-w], ((0, 0), (w, 0), (0, 0)))
        mean = (c - lag) / jnp.minimum(t, float(w))[None, :, None]
        outs.append(mean - pg[:, :, g])
    d = jnp.stack(outs, axis=2).astype(p.dtype)
    y = jnp.einsum('bsgc,gcd->bsgd', d, pool_w)
    return y.reshape(B, S, POOL_W) * pool_scale


def mlstm_chunkwise(q, k, v, log_i, log_f):
    B, S, H, dk = q.shape
    dv = v.shape[-1]
    nc = S // CHUNK

    def to_chunks(a):
        a = a.reshape((B, nc, CHUNK) + a.shape[2:])
        return jnp.moveaxis(jnp.moveaxis(a, 1, 0), 3, 2)

    qc, kc, vc, lic, lfc = (to_chunks(a) for a in (q, k, v, log_i, log_f))
    tril = jnp.tril(jnp.ones((CHUNK, CHUNK), dtype=bool))

    def step(carry, xs):
        C, n, m = carry
        qb, kb, vb, li, lf = xs
        b = jnp.cumsum(lf, axis=-1)
        Dm = b[..., :, None] - b[..., None, :] + li[..., None, :]
        Dm = jnp.where(tril, Dm, -jnp.inf)
        inter = b + m[..., None]
        m_t = jnp.maximum(inter, jnp.max(Dm, axis=-1))
        W = jnp.exp(Dm - m_t[..., None]) * jnp.einsum('bhtd,bhsd->bhts', qb, kb)
        iw = jnp.exp(inter - m_t)
        num = iw[..., None] * jnp.einsum('bhtk,bhkv->bhtv', qb, C) + jnp.einsum('bhts,bhsv->bhtv', W, vb)
        den = iw * jnp.einsum('bhtk,bhk->bht', qb, n) + jnp.sum(W, axis=-1)
        h = num / jnp.maximum(jnp.abs(den), jnp.exp(-m_t))[..., None]
        bL = b[..., -1]
        g = bL[..., None] - b + li
        m_new = jnp.maximum(bL + m, jnp.max(g, axis=-1))
        decay = jnp.exp(bL + m - m_new)
        ws = jnp.exp(g - m_new[..., None])
        C_new = decay[..., None, None] * C + jnp.einsum('bhs,bhsk,bhsv->bhkv', ws, kb, vb)
        n_new = decay[..., None] * n + jnp.einsum('bhs,bhsk->bhk', ws, kb)
        return (C_new, n_new, m_new), h

    init = (jnp.zeros((B, H, dk, dv), jnp.float32),
            jnp.zeros((B, H, dk), jnp.float32),
            jnp.zeros((B, H), jnp.float32))
    _, hs = lax.scan(step, init, (qc, kc, vc, lic, lfc))
    return jnp.transpose(hs, (1, 0, 3, 2, 4)).reshape(B, S, H, dv)


def token_mix(u, w_in, gate_bias, qk_conv, head_norm, pool_w, pool_scale, w_out):
    B, S, _ = u.shape
    H, dh = MLSTM_HEADS, MLSTM_HEAD_DIM
    z = u @ w_in
    o1 = POOL_W
    o2 = o1 + 2 * MLSTM_W
    o3 = o2 + MLSTM_W
    o4 = o3 + MLSTM_W
    o5 = o4 + H
    p, qk, v, og, gi, gf = z[..., :o1], z[..., o1:o2], z[..., o2:o3], z[..., o3:o4], z[..., o4:o5], z[..., o5:]
    y_pool = multiscale_pool(p, pool_w, pool_scale)
    qk = jax.nn.silu(causal_dwconv(qk, qk_conv)).astype(jnp.float32)
    q = qk[..., :MLSTM_W].reshape(B, S, H, dh) * (dh ** -0.5)
    k = qk[..., MLSTM_W:].reshape(B, S, H, dh)
    vh = v.astype(jnp.float32).reshape(B, S, H, dh)
    gb = gate_bias.astype(jnp.float32)
    log_i = gi.astype(jnp.float32) + gb[:H]
    log_f = jax.nn.log_sigmoid(gf.astype(jnp.float32) + gb[H:])
    h = mlstm_chunkwise(q, k, vh, log_i, log_f)
    h = h * lax.rsqrt(jnp.mean(h * h, axis=-1, keepdims=True) + EPS)
    h = h * head_norm.astype(jnp.float32).reshape(H, dh)
    y_mlstm = h.reshape(B, S, MLSTM_W).astype(u.dtype) * jax.nn.sigmoid(og)
    return jnp.concatenate([y_pool, y_mlstm], axis=-1) @ w_out


def cross_attention(u, mem_n, wq, wkv, wo):
    B, S, _ = u.shape
    q = (u @ wq).reshape(B, S, XATTN_HEADS, XATTN_HEAD_DIM)
    kv = (mem_n @ wkv).reshape(B, mem_n.shape[1], 2, XATTN_HEADS, XATTN_HEAD_DIM)
    k, v = kv[:, :, 0], kv[:, :, 1]
    s = jnp.einsum('bshd,bmhd->bhsm', q, k).astype(jnp.float32) * (XATTN_HEAD_DIM ** -0.5)
    pr = jax.nn.softmax(s, axis=-1).astype(v.dtype)
    o = jnp.einsum('bhsm,bmhd->bshd', pr, v).reshape(B, S, D_MODEL)
    return o @ wo


def setup_inputs(seed: int = 0) -> dict:
    key = jax.random.key(seed)
    ks = jax.random.split(key, 32)
    L, D, F = DEPTH, D_MODEL, D_FF

    def dense(k, shape, fan_in):
        return jax.random.normal(k, shape, jnp.float32) * (fan_in ** -0.5)

    def gain(k, shape):
        return 1.0 + 0.05 * jax.random.normal(k, shape, jnp.float32)

    f_bias = jnp.linspace(3.0, 6.0, MLSTM_HEADS, dtype=jnp.float32)
    gate_bias = jnp.concatenate([
        0.1 * jax.random.normal(ks[20], (L, MLSTM_HEADS), jnp.float32),
        f_bias[None, :] + 0.1 * jax.random.normal(ks[21], (L, MLSTM_HEADS), jnp.float32)], axis=-1)
    return {
        "x": jax.random.normal(ks[0], (BATCH, SEQ, D), jnp.float32),
        "mem": jax.random.normal(ks[1], (BATCH, N_MEM, D), jnp.float32),
        "ffn1_norm": gain(ks[2], (L, D)),
        "ffn1_w_gate": dense(ks[3], (L, D, F), D),
        "ffn1_w_up": dense(ks[4], (L, D, F), D),
        "ffn1_w_down": dense(ks[5], (L, F, D), F),
        "mix_norm": gain(ks[6], (L, D)),
        "w_in": dense(ks[7], (L, D, D_IN), D),
        "gate_bias": gate_bias,
        "qk_conv": dense(ks[8], (L, CONV_W, 2 * MLSTM_W), CONV_W),
        "head_norm": gain(ks[9], (L, MLSTM_W)),
        "pool_w": dense(ks[10], (L, POOL_GROUPS, POOL_GROUP_W, POOL_GROUP_W), POOL_GROUP_W),
        "pool_scale": gain(ks[11], (L, POOL_W)),
        "w_out": dense(ks[12], (L, D_MIX, D), D_MIX),
        "xattn_norm": gain(ks[13], (L, D)),
        "mem_norm": gain(ks[14], (L, D)),
        "xattn_wq": dense(ks[15], (L, D, D), D),
        "xattn_wkv": dense(ks[16], (L, D, 2 * D), D),
        "xattn_wo": dense(ks[17], (L, D, D), D),
        "ffn2_norm": gain(ks[18], (L, D)),
        "ffn2_w_gate": dense(ks[19], (L, D, F), D),
        "ffn2_w_up": dense(ks[22], (L, D, F), D),
        "ffn2_w_down": dense(ks[23], (L, F, D), F),
        "final_norm": gain(ks[24], (D,)),
    }


def reference(x, mem, ffn1_norm, ffn1_w_gate, ffn1_w_up, ffn1_w_down, mix_norm, w_in, gate_bias,
              qk_conv, head_norm, pool_w, pool_scale, w_out, xattn_norm, mem_norm, xattn_wq,
              xattn_wkv, xattn_wo, ffn2_norm, ffn2_w_gate, ffn2_w_up, ffn2_w_down, final_norm):
    h = x
    for l in range(DEPTH):
        h = h + 0.5 * swiglu(rms_norm(h, ffn1_norm[l]), ffn1_w_gate[l], ffn1_w_up[l], ffn1_w_down[l])
        h = h + token_mix(rms_norm(h, mix_norm[l]), w_in[l], gate_bias[l], qk_conv[l], head_norm[l],
                          pool_w[l], pool_scale[l], w_out[l])
        h = h + cross_attention(rms_norm(h, xattn_norm[l]), rms_norm(mem, mem_norm[l]),
                                xattn_wq[l], xattn_wkv[l], xattn_wo[l])
        h = h + 0.5 * swiglu(rms_norm(h, ffn2_norm[l]), ffn2_w_gate[l], ffn2_w_up[l], ffn2_w_down[l])
    return rms_norm(h, final_norm)
```

```python
import contextlib
import numpy as np
import concourse.bass as bass
import concourse.mybir as mybir
from concourse.bass_utils import run_bass_kernel_spmd

F32 = mybir.dt.float32
BF16 = mybir.dt.bfloat16
ALU = mybir.AluOpType
AF = mybir.ActivationFunctionType

PE, ACT, DVE, POOL, SP = "pe", "act", "dve", "pool", "sp"
ENGS = (PE, ACT, DVE, POOL, SP)

D = 2048
KC = 16
FF = 5632
DIN = 5128
NMEM = 256
EPS = 1e-6
PL = 168
SB_BASE = 16640
SB_END = 229376
STRICT = True


class Ev:
    __slots__ = ("key", "ins", "val")

    def __init__(self, key, ins=None, val=None):
        self.key, self.ins, self.val = key, ins, val


class Ins:
    __slots__ = ("eng", "fn", "deps", "need_inc", "semval", "dma_sem", "tag")

    def __init__(self, eng, fn):
        self.eng, self.fn = eng, fn
        self.tag = Prog.cur_tag
        self.deps = []
        self.need_inc = False
        self.semval = None
        self.dma_sem = None


class Tl:
    __slots__ = ("t", "name", "lw", "rd", "dsem", "inh")

    def __init__(self, t, name):
        self.t, self.name = t, name
        self.lw = None
        self.rd = {}
        self.dsem = None
        self.inh = []

    def __getitem__(self, idx):
        return self.t[idx]


class Prog:
    cur_tag = ""

    def __init__(self, nc):
        self.nc = nc
        self.q = {e: [] for e in ENGS}
        self.es = contextlib.ExitStack()
        self.dsems = {}
        self.nd = 0
        self.last = {e: None for e in ENGS}
        self.sp_ptr = SB_BASE
        self.nalloc = 0
        self.live = []
        self.dead = []

    def sb(self, name, shape, dt):
        n = 1
        for s in shape[1:]:
            n *= s
        nbytes = n * (4 if dt == F32 else 2)
        nbytes = (nbytes + 63) // 64 * 64
        off = self.sp_ptr
        assert off + nbytes <= SB_END, "SBUF overflow at %s (%d)" % (name, off + nbytes)
        self.sp_ptr += nbytes
        self.nalloc += 1
        t = self.nc.alloc_sbuf_tensor_at("%s_%d" % (name, self.nalloc), list(shape), dt, offset=off)
        tl = Tl(t, name)
        end = off + nbytes
        keep = []
        for (o, e_, evs) in self.dead:
            if o < end and off < e_:
                tl.inh.extend(evs)
                if off <= o and e_ <= end:
                    continue
            keep.append((o, e_, evs))
        self.dead = keep
        self.live.append((off, end, tl))
        return tl

    def mark(self):
        return self.sp_ptr

    def release(self, m):
        keep = []
        for (o, e_, tl) in self.live:
            if o >= m:
                evs = list(tl.inh) + list(tl.rd.values()) + ([tl.lw] if tl.lw is not None else [])
                if evs:
                    self.dead.append((o, e_, evs))
            else:
                keep.append((o, e_, tl))
        self.live = keep
        self.sp_ptr = m

    def ps(self, name, shape, dt=F32):
        t = self.es.enter_context(self.nc.psum_tensor(name, list(shape), dt))
        return Tl(t, name)

    def _dsem(self, tl):
        if tl.dsem is None:
            key = "d_" + tl.name
            if key not in self.dsems:
                h = self.es.enter_context(self.nc.semaphore(key))
                self.dsems[key] = [h, 0]
            tl.dsem = key
        return tl.dsem

    def _deps(self, ins, reads, writes):
        eng = ins.eng
        deps = []
        for tl in reads:
            if tl.lw is not None:
                deps.append(tl.lw)
        for tl in writes:
            if tl.lw is not None:
                deps.append(tl.lw)
            for k, ev in tl.rd.items():
                if k == eng and not STRICT:
                    continue
                deps.append(ev)
        for tl in list(reads) + list(writes):
            if tl.inh:
                for ev in tl.inh:
                    if ev.key != eng or STRICT:
                        deps.append(ev)
        for tl in writes:
            tl.inh = []
        out = []
        for ev in deps:
            if ev.ins is ins:
                continue
            if ev.key == PE and eng == PE:
                continue
            out.append(ev)
            if ev.ins is not None:
                ev.ins.need_inc = True
        ins.deps = out

    def op(self, eng, fn, reads=(), writes=()):
        ins = Ins(eng, fn)
        self._deps(ins, reads, writes)
        ev = Ev(eng, ins)
        for tl in reads:
            tl.rd[eng] = ev
        for tl in writes:
            tl.lw = ev
            tl.rd = {}
        self.q[eng].append(ins)
        self.last[eng] = ev
        return ins

    def dma(self, queue, dst, pairs, src=None):
        key = self._dsem(dst)
        rec = self.dsems[key]

        def fn(e, pairs=pairs, h=rec[0]):
            r = None
            for (o, i) in pairs:
                r = e.dma_start(out=o, in_=i).then_inc(h, 16)
            return r

        ins = Ins(queue, fn)
        ins.dma_sem = key
        rds = [src] if src is not None else []
        self._deps(ins, rds, [dst])
        if rec[1] > 0:
            ins.deps.append(Ev(key, None, rec[1]))
        rec[1] += 16 * len(pairs)
        ev = Ev(key, None, rec[1])
        for tl in rds:
            tl.rd[key] = ev
        dst.lw = ev
        dst.rd = {}
        self.q[queue].append(ins)
        return ins

    def barrier(self, engs=(PE, ACT, DVE, SP), final=False):
        evs = [self.last[e] for e in engs if self.last[e] is not None]
        devs = [Ev(k, None, rec[1]) for k, rec in self.dsems.items() if rec[1] > 0 and (final or not k.startswith("d_ring"))]
        for e in engs:
            ins = Ins(e, None)
            ins.deps = [ev for ev in evs if ev.key != e] + devs
            for ev in ins.deps:
                if ev.ins is not None:
                    ev.ins.need_inc = True
            self.q[e].append(ins)

    def emit(self):
        nc = self.nc
        sems = {}
        for e in ENGS:
            sems[e] = self.es.enter_context(nc.semaphore("eng_" + e))
            c = 0
            for ins in self.q[e]:
                if ins.dma_sem is None and ins.need_inc:
                    c += 1
                    ins.semval = c
        self.stats = {e: [len(self.q[e]), 0] for e in ENGS}

        def handle(key):
            return sems[key] if key in sems else self.dsems[key][0]

        self.emitted = {e: [] for e in ENGS}

        def run(eng_name, eobj):
            seen = {}
            em = self.emitted[eng_name]
            for ins in self.q[eng_name]:
                need = {}
                for ev in ins.deps:
                    v = ev.val if ev.ins is None else ev.ins.semval
                    if seen.get(ev.key, 0) >= v:
                        continue
                    if need.get(ev.key, 0) < v:
                        need[ev.key] = v
                for k, v in need.items():
                    eobj.wait_ge(handle(k), v)
                    em.append(("W", ins.tag, k))
                    seen[k] = v
                    self.stats[eng_name][1] += 1
                if ins.fn is None:
                    continue
                r = ins.fn(eobj)
                em.append(("I", ins.tag, ""))
                if ins.dma_sem is None and ins.need_inc:
                    r.then_inc(sems[eng_name], 1)

        with nc.Block() as block:
            @block.tensor
            def _(e):
                run(PE, e)

            @block.scalar
            def _(e):
                run(ACT, e)

            @block.vector
            def _(e):
                run(DVE, e)

            @block.gpsimd
            def _(e):
                run(POOL, e)

            @block.sync
            def _(e):
                run(SP, e)


def build(NSEQ, S, T, DEPTH):
    nc = bass.Bass("TRN2", target_bir_lowering=False)
    P = Prog(nc)
    NTT = T // 128
    NPASS = S // T
    L = DEPTH

    def din(name, shape):
        return nc.dram_tensor(name, list(shape), F32, kind="ExternalInput").ap()

    x_d = din("x", [NSEQ * S, D])
    mem_d = din("mem", [NSEQ * NMEM, D])
    w = {}
    for f in ("ffn1", "ffn2"):
        w[f + "_g"] = din(f + "_w_gate", [L * D, FF])
        w[f + "_u"] = din(f + "_w_up", [L * D, FF])
        w[f + "_d"] = din(f + "_w_down", [L * FF, D])
    w["in"] = din("w_in", [L * D, DIN])
    w["out"] = din("w_out", [L * D, D])
    w["pool"] = din("pool_w", [L * 1024, 256])
    w["wq"] = din("xattn_wq", [L * D, D])
    w["wkv"] = din("xattn_wkv", [L * D, 2 * D])
    w["wo"] = din("xattn_wo", [L * D, D])
    NPRM = L * PL + 16
    prm_d = din("prm", [128, NPRM])
    cst_d = din("cst", [128, 448])
    out_d = nc.dram_tensor("out", [NSEQ * S, D], F32, kind="ExternalOutput").ap()
    out_tl = [Tl(out_d, "out%d" % i) for i in range(2)]

    prm = P.sb("prm", [128, NPRM], F32)
    cst = P.sb("cst", [128, 448], F32)
    ident_f = lambda: cst[:, 0:128]
    triu_f = lambda: cst[:, 128:256]
    ones_f = lambda: cst[:, 256:384]
    cbf = P.sb("cbf", [128, 384], BF16)
    ident_b = lambda: cbf[:, 0:128]
    mask_b = lambda: cbf[:, 128:256]
    ones_b = lambda: cbf[:, 256:384]
    h = [P.sb("h%d" % k, [128, T], F32) for k in range(KC)]
    Cst = [[P.sb("Cst%d_%d" % (l, hd), [128, 2, 257], F32) for hd in range(4)] for l in range(L)]
    ztail = [[P.sb("zt%d_%d" % (l, c), [128, 3], F32) for c in range(16)] for l in range(L)]
    ptail = [[P.sb("pt%d_%d" % (l, c), [128, 16], F32) for c in range(8)] for l in range(L)]
    NR = 6
    ring = [P.sb("ring%d" % i, [128, 4096], BF16) for i in range(NR)]
    rstate = [0]
    NBLK = L * 222
    wscr = [nc.dram_tensor("wscr%d" % l_, [222, 128, 4096], BF16, kind="Internal").ap() for l_ in range(L)]
    wst = [Tl(wscr[0], "wst%d" % i) for i in range(NR)]
    blk = [0]
    passno = [0]
    kvscr = nc.dram_tensor("kvscr", [L * 2, 128, 4096], BF16, kind="Internal").ap()
    kvs_k = [Tl(kvscr, "kvk%d" % l_) for l_ in range(L)]
    kvs_v = [Tl(kvscr, "kvv%d" % l_) for l_ in range(L)]
    psf = [P.ps("psf%d" % i, [128, 512], F32) for i in range(6)]
    psb = [P.ps("psb%d" % i, [128, 512], BF16) for i in range(2)]
    pstate = [0, 0]

    def nps():
        pstate[0] += 1
        return psf[pstate[0] % 6]

    def npb():
        pstate[1] += 1
        return psb[pstate[1] % 2]

    rr = [0]

    def ev_eng():
        rr[0] += 1
        return ACT if rr[0] % 2 else DVE

    def mm(ps, out_ap, lhsT, rhs, start, stop, reads):
        P.op(PE, lambda e: e.matmul(out_ap, lhsT, rhs, start=start, stop=stop), reads=reads, writes=[ps])

    def tp(ps, out_ap, in_ap, ident, reads):
        P.op(PE, lambda e: e.transpose(out_ap, in_ap, ident), reads=reads, writes=[ps])

    def wload(wd, r0, nk, c0, bw):
        si = rstate[0] % NR
        sl = ring[si]
        rstate[0] += 1
        b = blk[0]
        blk[0] += 1
        scr = wscr[b // 222][b % 222][:, 0:nk * bw]
        sv = sl[:, 0:nk * bw].rearrange("p (k c) -> p k c", k=nk)
        if passno[0] == 0:
            src = wd[r0:r0 + nk * 128, c0:c0 + bw].rearrange("(r p) c -> p r c", p=128)
            P.dma(POOL, sl, [(sv, src)])
            if NSEQ * NPASS > 1:
                P.dma(SP, wst[si], [(scr, sl[:, 0:nk * bw])], src=sl)
        else:
            P.dma(POOL, sl, [(sl[:, 0:nk * bw], scr)])
        return sl, sv

    def gemm_fm(wd, r0, nk, c0, ncols, rhs, rhs_tl, n, evac):
        for cb in range(0, ncols, 256):
            bw = min(256, ncols - cb)
            sl, sv = wload(wd, r0, nk, c0 + cb, bw)
            for dj in range(bw // 128):
                ps = nps()
                for k in range(nk):
                    mm(ps, ps[:, 0:n], sv[:, k, dj * 128:(dj + 1) * 128], rhs(k), k == 0, k == nk - 1,
                       [sl, rhs_tl[k]])
                evac(cb // 128 + dj, ps)

    def gemm_tm(wd, r0, nk, c0, ncols, lhs, lhs_tl, ntile, evac):
        for cb in range(0, ncols, 256):
            bw = min(256, ncols - cb)
            sl, sv = wload(wd, r0, nk, c0 + cb, bw)
            for tt in range(ntile):
                ps = nps()
                for k in range(nk):
                    mm(ps, ps[:, 0:bw], lhs(k, tt), sv[:, k, 0:bw], k == 0, k == nk - 1, [sl, lhs_tl[k]])
                evac(cb, bw, tt, ps)

    def rsqrt_chain(buf, ap, ap2):
        P.op(ACT, lambda e: e.activation(ap, ap2, AF.Sqrt), reads=[buf], writes=[buf])
        P.op(DVE, lambda e: e.reciprocal(ap, ap2), reads=[buf], writes=[buf])

    def rmsnorm(gcol, xn, inplace=False):
        old_tag = Prog.cur_tag
        Prog.cur_tag = old_tag + ".norm"
        m = P.mark()
        sq = [P.sb("sq%d" % i, [128, T], BF16) for i in range(3)]
        rs = P.sb("rs", [128, T], F32)
        ps = nps()
        for k in range(KC):
            s_ = sq[k % 3]
            P.op(ACT, lambda e, s_=s_, k=k: e.activation(s_[:, :], h[k][:, :], AF.Square), reads=[h[k]], writes=[s_])
            mm(ps, ps[:, 0:T], ones_b(), s_[:, :], k == 0, k == KC - 1, [s_, cbf])
        P.op(DVE, lambda e: e.tensor_scalar(rs[:, :], ps[:, 0:T], 1.0 / D, EPS, ALU.mult, ALU.add), reads=[ps], writes=[rs])
        rsqrt_chain(rs, rs[:, :], rs[:, :])
        for k in range(KC):
            dst = h[k] if inplace else xn[k]
            P.op(DVE, lambda e, k=k, dst=dst: e.scalar_tensor_tensor(
                dst[:, :], h[k][:, :], prm[:, gcol + k:gcol + k + 1], rs[:, :], ALU.mult, ALU.mult),
                reads=[h[k], rs, prm], writes=[dst])
        P.release(m)
        Prog.cur_tag = old_tag

    def resid_add(scale):
        def evac(j, ps):
            P.op(DVE, lambda e: e.scalar_tensor_tensor(h[j][:, :], ps[:, 0:T], scale, h[j][:, :], ALU.mult, ALU.add),
                 reads=[ps, h[j]], writes=[h[j]])
        return evac

    def ffn(l, name, gcol):
        Prog.cur_tag = "ffn"
        m = P.mark()
        xn = [P.sb("xn%d" % k, [128, T], BF16) for k in range(KC)]
        hT = [P.sb("hT%d" % k, [128, T], BF16) for k in range(FF // 128)]
        rmsnorm(gcol, xn)
        Prog.cur_tag = "ffn.gu"
        m2 = P.mark()
        stmp = [P.sb("stmp%d" % i, [128, T], F32) for i in range(3)]
        sc = [0]
        wg, wu, wd = w[name + "_g"], w[name + "_u"], w[name + "_d"]
        for cb in range(0, FF, 256):
            tmps = {}

            def ev_g(j, ps, tmps=tmps):
                t_ = stmp[sc[0] % 3]
                sc[0] += 1
                tmps[j] = t_
                P.op(ACT, lambda e: e.activation(t_[:, :], ps[:, 0:T], AF.Silu), reads=[ps], writes=[t_])

            def ev_u(j, ps, tmps=tmps):
                t_ = tmps[j]
                P.op(DVE, lambda e: e.tensor_tensor(hT[j][:, :], t_[:, :], ps[:, 0:T], ALU.mult), reads=[ps, t_], writes=[hT[j]])

            base = cb // 128
            gemm_fm(wg, l * D, KC, cb, 256, lambda k: xn[k][:, :], xn, T, lambda j, ps: ev_g(base + j, ps))
            gemm_fm(wu, l * D, KC, cb, 256, lambda k: xn[k][:, :], xn, T, lambda j, ps: ev_u(base + j, ps))
        P.release(m2)
        Prog.cur_tag = "ffn.down"
        for g0 in range(0, 44, 11):
            gemm_fm(wd, l * FF + g0 * 128, 11, 0, D, lambda k, g0=g0: hT[g0 + k][:, :], hT[g0:g0 + 11], T, resid_add(0.5))
        P.release(m)

    def mixer(l, first, tok0):
        pb = l * PL
        Prog.cur_tag = "mix"
        m = P.mark()
        xn = [P.sb("xn%d" % k, [128, T], BF16) for k in range(KC)]
        rmsnorm(pb + 16, xn)
        Prog.cur_tag = "mix.pool"
        xr = lambda k: xn[k][:, :]
        m1 = P.mark()
        dT = [P.sb("dT%d" % c, [128, T], BF16) for c in range(8)]
        ypT = [P.sb("ypT%d" % c, [128, T], BF16) for c in range(8)]
        pbuf = [P.sb("pbuf%d" % i, [128, 16 + T], F32) for i in range(2)]
        pa = [P.sb("pa%d" % i, [128, 16 + T], F32) for i in range(2)]
        pc_ = [0]

        def ev_p(c, ps):
            X = pbuf[pc_[0] % 2]
            pc_[0] += 1
            wlog = c // 2 + 1
            wsz = 1 << wlog
            if first:
                P.op(DVE, lambda e: e.memset(X[:, 0:16], 0.0), writes=[X])
            else:
                P.op(DVE, lambda e: e.tensor_copy(X[:, 0:16], ptail[l][c][:, :]), reads=[ptail[l][c]], writes=[X])
            P.op(ACT, lambda e: e.activation(X[:, 16:16 + T], ps[:, 0:T], AF.Copy), reads=[ps], writes=[X])
            P.op(ACT, lambda e: e.activation(ptail[l][c][:, :], X[:, T:T + 16], AF.Copy), reads=[X], writes=[ptail[l][c]])
            src, lo = X, 0
            for i in range(wlog):
                sh = 1 << i
                dst = pa[i % 2]
                nlo = lo + sh
                P.op(DVE, lambda e, src=src, dst=dst, nlo=nlo, sh=sh: e.tensor_tensor(
                    dst[:, nlo:16 + T], src[:, nlo:16 + T], src[:, nlo - sh:16 + T - sh], ALU.add),
                    reads=[src], writes=[dst])
                src, lo = dst, nlo
            A = src
            P.op(DVE, lambda e: e.scalar_tensor_tensor(dT[c][:, :], A[:, 16:16 + T], 1.0 / wsz, X[:, 16:16 + T],
                                                       ALU.mult, ALU.subtract), reads=[A, X], writes=[dT[c]])
            if first:
                g = c // 2
                P.op(DVE, lambda e: e.tensor_tensor(A[:, 16:32], A[:, 16:32], cst[:, 384 + g * 16:384 + (g + 1) * 16], ALU.mult),
                     reads=[A, cst], writes=[A])
                P.op(DVE, lambda e: e.tensor_tensor(dT[c][:, 0:16], A[:, 16:32], X[:, 16:32], ALU.subtract),
                     reads=[A, X], writes=[dT[c]])

        gemm_fm(w["in"], l * D, KC, 0, 1024, xr, xn, T, ev_p)
        slp, svp = wload(w["pool"], l * 1024, 8, 0, 256)
        for g in range(4):
            for dc in range(2):
                ps = nps()
                for cc in range(2):
                    mm(ps, ps[:, 0:T], svp[:, 2 * g + cc, dc * 128:(dc + 1) * 128], dT[2 * g + cc][:, :], cc == 0, cc == 1,
                       [slp, dT[2 * g + cc]])
                j = 2 * g + dc
                P.op(ACT, lambda e, j=j, ps=ps: e.activation(ypT[j][:, :], ps[:, 0:T], AF.Copy,
                                                             scale=prm[:, pb + 144 + j:pb + 145 + j]),
                     reads=[ps, prm], writes=[ypT[j]])
        gemm_fm(w["out"], l * D, 8, 0, D, lambda k: ypT[k][:, :], ypT, T, resid_add(1.0))
        P.release(m1)
        Prog.cur_tag = "mix.qk"
        qkT = [P.sb("qkT%d" % c, [128, T], BF16) for c in range(16)]
        m2 = P.mark()
        zb = [P.sb("zb%d" % i, [128, 3 + T], F32) for i in range(2)]
        za = [P.sb("za%d" % i, [128, T], F32) for i in range(2)]
        zc = [0]

        def ev_qk(c, ps):
            Z = zb[zc[0] % 2]
            A = za[zc[0] % 2]
            zc[0] += 1
            if first:
                P.op(DVE, lambda e: e.memset(Z[:, 0:3], 0.0), writes=[Z])
            else:
                P.op(DVE, lambda e: e.tensor_copy(Z[:, 0:3], ztail[l][c][:, :]), reads=[ztail[l][c]], writes=[Z])
            P.op(ACT, lambda e: e.activation(Z[:, 3:3 + T], ps[:, 0:T], AF.Copy), reads=[ps], writes=[Z])
            P.op(ACT, lambda e: e.activation(ztail[l][c][:, :], Z[:, T:T + 3], AF.Copy), reads=[Z], writes=[ztail[l][c]])
            cw = pb + 80 + c * 4
            P.op(DVE, lambda e: e.tensor_scalar(A[:, :], Z[:, 0:T], prm[:, cw:cw + 1], 0.0, ALU.mult, ALU.add), reads=[Z, prm], writes=[A])
            for j in range(1, 4):
                P.op(DVE, lambda e, j=j: e.scalar_tensor_tensor(A[:, :], Z[:, j:j + T], prm[:, cw + j:cw + j + 1], A[:, :],
                                                                ALU.mult, ALU.add), reads=[Z, A, prm], writes=[A])
            P.op(ACT, lambda e: e.activation(qkT[c][:, :], A[:, :], AF.Silu), reads=[A], writes=[qkT[c]])

        gemm_fm(w["in"], l * D, KC, 1024, 2048, xr, xn, T, ev_qk)
        P.release(m2)
        Prog.cur_tag = "mix.vog"
        vaug = [P.sb("vaug%d" % tt, [128, 4, 257], BF16) for tt in range(NTT)]
        G = [P.sb("G%d" % tt, [128, 1024], BF16) for tt in range(NTT)]
        gsc = [P.sb("gsc%d" % tt, [128, 12], F32) for tt in range(NTT)]
        gtmp = [P.sb("gtmp%d" % tt, [128, 16], F32) for tt in range(NTT)]
        for tt in range(NTT):
            P.op(DVE, lambda e, tt=tt: e.memset(vaug[tt][:, :, 256:257], 1.0), writes=[vaug[tt]])
        xl = lambda k, tt: xn[k][:, tt * 128:(tt + 1) * 128]

        def ev_v(cb, bw, tt, ps):
            hd = cb // 256
            eng = ev_eng()
            if eng == ACT:
                P.op(ACT, lambda e: e.activation(vaug[tt][:, hd, 0:256], ps[:, 0:256], AF.Copy), reads=[ps], writes=[vaug[tt]])
            else:
                P.op(DVE, lambda e: e.tensor_copy(vaug[tt][:, hd, 0:256], ps[:, 0:256]), reads=[ps], writes=[vaug[tt]])

        def ev_og(cb, bw, tt, ps):
            P.op(ACT, lambda e: e.activation(G[tt][:, cb:cb + bw], ps[:, 0:bw], AF.Sigmoid), reads=[ps], writes=[G[tt]])

        def ev_gate(cb, bw, tt, ps):
            gt, gs = gtmp[tt], gsc[tt]
            gb = pb + 160
            P.op(DVE, lambda e: e.tensor_tensor(gt[:, 0:8], ps[:, 0:8], prm[:, gb:gb + 8], ALU.add), reads=[ps, prm], writes=[gt])
            P.op(ACT, lambda e: e.activation(gt[:, 8:12], gt[:, 4:8], AF.Exp, scale=-1.0), reads=[gt], writes=[gt])
            P.op(ACT, lambda e: e.activation(gt[:, 8:12], gt[:, 8:12], AF.Ln, bias=1.0), reads=[gt], writes=[gt])
            gate_pending.append((gt, gs))

        def gate_finish(gt, gs):
            pg = nps()
            mm(pg, pg[:, 0:4], triu_f(), gt[:, 8:12], True, True, [cst, gt])
            mm(pg, pg[:, 4:8], ones_f(), gt[:, 8:12], True, True, [cst, gt])
            P.op(DVE, lambda e: e.tensor_tensor(gt[:, 12:16], gt[:, 0:4], pg[:, 0:4], ALU.add), reads=[gt, pg], writes=[gt])
            P.op(ACT, lambda e: e.activation(gs[:, 0:4], gt[:, 12:16], AF.Exp), reads=[gt], writes=[gs])
            P.op(ACT, lambda e: e.activation(gs[:, 4:12], pg[:, 0:8], AF.Exp, scale=-1.0), reads=[pg], writes=[gs])

        gate_pending = []
        gemm_tm(w["in"], l * D, KC, 5120, 8, xl, xn, NTT, ev_gate)
        gemm_tm(w["in"], l * D, KC, 3072, 1024, xl, xn, NTT, ev_v)
        for (gt_, gs_) in gate_pending:
            gate_finish(gt_, gs_)
        gemm_tm(w["in"], l * D, KC, 4096, 1024, xl, xn, NTT, ev_og)
        Prog.cur_tag = "mix.mlstm"
        ymT = [P.sb("ymT%d" % c, [128, T], BF16) for c in range(8)]
        Cb = [P.sb("Cb%d" % hd, [128, 2, 257], BF16) for hd in range(4)]
        Wp = [P.sb("Wp%d" % i, [128, 128], BF16) for i in range(2)]
        kc = [P.sb("kc%d" % i, [128, 256], BF16) for i in range(2)]
        ytok = [P.sb("ytok%d" % i, [128, 256], BF16) for i in range(2)]
        junk = P.sb("junk", [128, 256], F32)
        scs = [P.sb("scs%d" % i, [128, 40], F32) for i in range(2)]
        if first:
            for hd in range(4):
                P.op(DVE, lambda e, hd=hd: e.memset(Cst[l][hd][:, :, :], 0.0), writes=[Cst[l][hd]])
        for hd in range(4):
            P.op(ACT, lambda e, hd=hd: e.activation(Cb[hd][:, :, :], Cst[l][hd][:, :, :], AF.Copy), reads=[Cst[l][hd]], writes=[Cb[hd]])
        def stA(tt, hd):
            ts = slice(tt * 128, (tt + 1) * 128)
            gs = gsc[tt]
            pS = psf[4]
            for i in range(2):
                mm(pS, pS[:, 0:128], qkT[8 + 2 * hd + i][:, ts], qkT[2 * hd + i][:, ts], i == 0, i == 1,
                   [qkT[8 + 2 * hd + i], qkT[2 * hd + i]])
            W_ = Wp[hd % 2]
            P.op(DVE, lambda e: e.scalar_tensor_tensor(W_[:, :], pS[:, 0:128], gs[:, hd:hd + 1], mask_b(), ALU.mult, ALU.mult),
                 reads=[pS, gs, cbf], writes=[W_])
            pk = npb()
            for i in range(2):
                tp(pk, pk[:, i * 128:(i + 1) * 128], qkT[8 + 2 * hd + i][:, ts], ident_b(), [qkT[8 + 2 * hd + i], cbf])
            kc_ = kc[hd % 2]
            P.op(ACT, lambda e: e.activation(kc_[:, :], pk[:, 0:256], AF.Copy, scale=gs[:, hd:hd + 1]), reads=[pk, gs], writes=[kc_])

        def stB(tt, hd):
            ts = slice(tt * 128, (tt + 1) * 128)
            gs = gsc[tt]
            sc = scs[tt % 2]
            W_ = Wp[hd % 2]
            kc_ = kc[hd % 2]
            pn = psf[hd]
            mm(pn, pn[:, 0:257], W_[:, :], vaug[tt][:, hd, :], True, False, [W_, vaug[tt]])
            for i in range(2):
                mm(pn, pn[:, 0:257], qkT[2 * hd + i][:, ts], Cb[hd][:, i, :], False, i == 1, [qkT[2 * hd + i], Cb[hd]])
            P.op(DVE, lambda e: e.tensor_tensor(sc[:, hd:hd + 1], pn[:, 256:257], gs[:, 4 + hd:5 + hd], ALU.mult),
                 reads=[pn, gs], writes=[sc])
            P.op(ACT, lambda e: e.activation(junk[:, :], pn[:, 0:256], AF.Square, accum_out=sc[:, 4 + hd:5 + hd]),
                 reads=[pn], writes=[junk, sc])
            for i in range(2):
                pd = psf[5 - i]
                mm(pd, pd[:, 0:257], kc_[:, i * 128:(i + 1) * 128], vaug[tt][:, hd, :], True, True, [kc_, vaug[tt]])
                P.op(DVE, lambda e, pd=pd, i=i: e.tensor_tensor(Cst[l][hd][:, i, :], pd[:, 0:257], Cst[l][hd][:, i, :], ALU.add),
                     reads=[pd, Cst[l][hd]], writes=[Cst[l][hd]])

        def fin(tt, a):
            ts = slice(tt * 128, (tt + 1) * 128)
            gs = gsc[tt]
            sc = scs[tt % 2]
            v2 = lambda o: sc[:, o + a:o + a + 2]
            P.op(ACT, lambda e: e.activation(v2(8), v2(0), AF.Abs), reads=[sc], writes=[sc])
            P.op(DVE, lambda e: e.tensor_scalar_max(v2(8), v2(8), 16.0), reads=[sc], writes=[sc])
            P.op(DVE, lambda e: e.reciprocal(v2(12), v2(8)), reads=[sc], writes=[sc])
            P.op(DVE, lambda e: e.tensor_tensor(v2(16), v2(12), gs[:, 4 + a:6 + a], ALU.mult), reads=[sc, gs], writes=[sc])
            P.op(DVE, lambda e: e.tensor_tensor(v2(20), v2(16), v2(16), ALU.mult), reads=[sc], writes=[sc])
            P.op(DVE, lambda e: e.tensor_tensor(v2(24), v2(20), v2(4), ALU.mult), reads=[sc], writes=[sc])
            P.op(DVE, lambda e: e.tensor_scalar(v2(28), v2(24), 1.0 / 256, EPS, ALU.mult, ALU.add), reads=[sc], writes=[sc])
            P.op(ACT, lambda e: e.activation(v2(32), v2(28), AF.Sqrt), reads=[sc], writes=[sc])
            P.op(DVE, lambda e: e.reciprocal(v2(32), v2(32)), reads=[sc], writes=[sc])
            P.op(DVE, lambda e: e.tensor_tensor(v2(36), v2(32), v2(16), ALU.mult), reads=[sc], writes=[sc])
            for hh in (a, a + 1):
                pn_ = psf[hh]
                y_ = ytok[hh % 2]
                P.op(DVE, lambda e, pn_=pn_, y_=y_, hh=hh: e.scalar_tensor_tensor(
                    y_[:, :], pn_[:, 0:256], sc[:, 36 + hh:37 + hh], G[tt][:, hh * 256:(hh + 1) * 256], ALU.mult, ALU.mult),
                    reads=[pn_, sc, G[tt]], writes=[y_])
                P.op(DVE, lambda e, hh=hh: e.tensor_scalar(Cst[l][hh][:, :, :], Cst[l][hh][:, :, :], gs[:, 8 + hh:9 + hh], 0.0, ALU.mult, ALU.add),
                     reads=[Cst[l][hh], gs], writes=[Cst[l][hh]])
                P.op(ACT, lambda e, hh=hh: e.activation(Cb[hh][:, :, :], Cst[l][hh][:, :, :], AF.Copy), reads=[Cst[l][hh]], writes=[Cb[hh]])

        def finP(tt, a):
            ts = slice(tt * 128, (tt + 1) * 128)
            for hh in (a, a + 1):
                y_ = ytok[hh % 2]
                py = npb()
                for i in range(2):
                    tp(py, py[:, i * 128:(i + 1) * 128], y_[:, i * 128:(i + 1) * 128], ident_b(), [y_, cbf])
                for i in range(2):
                    j = 2 * hh + i
                    P.op(ACT, lambda e, py=py, i=i, j=j: e.activation(
                        ymT[j][:, ts], py[:, i * 128:(i + 1) * 128], AF.Copy, scale=prm[:, pb + 152 + j:pb + 153 + j]),
                        reads=[py, prm], writes=[ymT[j]])

        pend = None
        for tt in range(NTT):
            stA(tt, 0)
            stA(tt, 1)
            stB(tt, 0)
            if pend is not None:
                finP(*pend)
            stA(tt, 2)
            stB(tt, 1)
            stA(tt, 3)
            fin(tt, 0)
            stB(tt, 2)
            stB(tt, 3)
            finP(tt, 0)
            fin(tt, 2)
            pend = (tt, 2)
        finP(*pend)
        Prog.cur_tag = "mix.wout2"
        gemm_fm(w["out"], l * D + 1024, 8, 0, D, lambda k: ymT[k][:, :], ymT, T, resid_add(1.0))
        P.release(m)

    def xattn(l, seq, first):
        pb = l * PL
        Prog.cur_tag = "xa"
        m = P.mark()
        xn = [P.sb("xn%d" % k, [128, T], BF16) for k in range(KC)]
        rmsnorm(pb + 32, xn)
        Prog.cur_tag = "xa.memq"
        KT = P.sb("KT", [128, KC, NMEM], BF16)
        V = P.sb("V", [128, 2, D], BF16)
        mnT = [P.sb("mnT%d" % c, [128, NMEM], BF16) for c in range(KC)] if first else None
        QT = [P.sb("QT%d" % c, [128, T], BF16) for c in range(KC)]
        qs = float(512 ** -0.5)
        m1 = P.mark()
        mst = P.sb("mst", [128, D], F32)
        mn = [P.sb("mn%d" % i, [128, D], BF16) for i in range(2)]
        mss = P.sb("mss", [128, 4], F32)
        if first:
            for mt in range(2):
                r0 = seq * NMEM + mt * 128
                P.dma(SP, mst, [(mst[:, :], mem_d[r0:r0 + 128, :])])
                P.op(ACT, lambda e, mt=mt: e.activation(mn[mt][:, :], mst[:, :], AF.Square, accum_out=mss[:, mt:mt + 1]),
                     reads=[mst], writes=[mn[mt], mss])
                P.op(DVE, lambda e, mt=mt: e.tensor_scalar(mss[:, 2 + mt:3 + mt], mss[:, mt:mt + 1], 1.0 / D, EPS, ALU.mult, ALU.add),
                     reads=[mss], writes=[mss])
                rsqrt_chain(mss, mss[:, 2 + mt:3 + mt], mss[:, 2 + mt:3 + mt])
                P.op(DVE, lambda e, mt=mt: e.tensor_scalar(mn[mt][:, :], mst[:, :], mss[:, 2 + mt:3 + mt], 0.0, ALU.mult, ALU.add),
                     reads=[mst, mss], writes=[mn[mt]])

        def ev_q(j, ps):
            P.op(ACT, lambda e: e.activation(QT[j][:, :], ps[:, 0:T], AF.Copy, scale=qs), reads=[ps], writes=[QT[j]])

        gemm_fm(w["wq"], l * D, KC, 0, D, lambda k: xn[k][:, :], xn, T, ev_q)
        if first:
            for mt in range(2):
                for k0 in range(0, KC, 4):
                    pt = npb()
                    for i in range(4):
                        tp(pt, pt[:, i * 128:(i + 1) * 128], mn[mt][:, (k0 + i) * 128:(k0 + i + 1) * 128], ident_b(), [mn[mt], cbf])
                    for i in range(4):
                        k = k0 + i
                        P.op(ACT, lambda e, pt=pt, i=i, k=k, mt=mt: e.activation(
                            mnT[k][:, mt * 128:(mt + 1) * 128], pt[:, i * 128:(i + 1) * 128], AF.Copy,
                            scale=prm[:, pb + 48 + k:pb + 49 + k]), reads=[pt, prm], writes=[mnT[k]])
        P.release(m1)

        def ev_k(j, ps):
            eng = ev_eng()
            if eng == ACT:
                P.op(ACT, lambda e: e.activation(KT[:, j, :], ps[:, 0:NMEM], AF.Copy), reads=[ps], writes=[KT])
            else:
                P.op(DVE, lambda e: e.tensor_copy(KT[:, j, :], ps[:, 0:NMEM]), reads=[ps], writes=[KT])

        def ev_vv(cb, bw, mt, ps):
            eng = ev_eng()
            if eng == ACT:
                P.op(ACT, lambda e: e.activation(V[:, mt, cb:cb + bw], ps[:, 0:bw], AF.Copy), reads=[ps], writes=[V])
            else:
                P.op(DVE, lambda e: e.tensor_copy(V[:, mt, cb:cb + bw], ps[:, 0:bw]), reads=[ps], writes=[V])

        Prog.cur_tag = "xa.kv"
        kview = kvscr[2 * l].rearrange("p (a b) -> p a b", a=KC)
        vview = kvscr[2 * l + 1].rearrange("p (a b) -> p a b", a=2)
        if first:
            gemm_fm(w["wkv"], l * D, KC, 0, D, lambda k: mnT[k][:, :], mnT, NMEM, ev_k)
            gemm_tm(w["wkv"], l * D, KC, D, D, lambda k, mt: mnT[k][:, mt * 128:(mt + 1) * 128], mnT, 2, ev_vv)
            if NPASS > 1:
                P.dma(SP, kvs_k[l], [(kview, KT[:, :, :])], src=KT)
                P.dma(SP, kvs_v[l], [(vview, V[:, :, :])], src=V)
        else:
            blk[0] += 16
            P.dma(SP, KT, [(KT[:, :, :], kview)], src=kvs_k[l])
            P.dma(SP, V, [(V[:, :, :], vview)], src=kvs_v[l])
        Prog.cur_tag = "xa.attn"
        oT = [P.sb("oT%d" % c, [128, T], BF16) for c in range(KC)]
        Pb = [P.sb("Pb%d" % i, [128, NMEM], BF16) for i in range(3)]
        PT = [P.sb("PT%d" % i, [128, 2, 128], BF16) for i in range(2)]
        otok = [P.sb("otok%d" % i, [128, 512], BF16) for i in range(3)]
        ast = [P.sb("ast%d" % i, [128, 4], F32) for i in range(4)]
        units = [(tt, hd) for tt in range(NTT) for hd in range(4)]
        pS_of, po_of = {}, {}

        def stA(ui_):
            tt, hd = units[ui_]
            ts = slice(tt * 128, (tt + 1) * 128)
            pS = nps()
            for i in range(4):
                mm(pS, pS[:, 0:NMEM], QT[4 * hd + i][:, ts], KT[:, 4 * hd + i, :], i == 0, i == 3, [QT[4 * hd + i], KT])
            st_ = ast[ui_ % 4]
            Pb_ = Pb[ui_ % 3]
            P.op(DVE, lambda e: e.reduce_max(st_[:, 0:1], pS[:, 0:NMEM], mybir.AxisListType.X), reads=[pS], writes=[st_])
            P.op(DVE, lambda e: e.tensor_scalar(st_[:, 1:2], st_[:, 0:1], -1.0, 0.0, ALU.mult, ALU.add), reads=[st_], writes=[st_])
            P.op(ACT, lambda e: e.activation(Pb_[:, :], pS[:, 0:NMEM], AF.Exp, bias=st_[:, 1:2], accum_out=st_[:, 2:3]),
                 reads=[pS, st_], writes=[Pb_, st_])
            P.op(DVE, lambda e: e.reciprocal(st_[:, 3:4], st_[:, 2:3]), reads=[st_], writes=[st_])

        def stB(ui_):
            tt, hd = units[ui_]
            Pb_ = Pb[ui_ % 3]
            PT_ = PT[ui_ % 2]
            pt = npb()
            for i in range(2):
                tp(pt, pt[:, i * 128:(i + 1) * 128], Pb_[:, i * 128:(i + 1) * 128], ident_b(), [Pb_, cbf])
            P.op(DVE, lambda e: e.tensor_copy(PT_[:, :, :], pt[:, 0:256].rearrange("p (a b) -> p a b", a=2)),
                 reads=[pt], writes=[PT_])

        def stB2(ui_):
            tt, hd = units[ui_]
            st_ = ast[ui_ % 4]
            PT_ = PT[ui_ % 2]
            ot_ = otok[ui_ % 3]
            po = nps()
            for mt in range(2):
                mm(po, po[:, 0:512], PT_[:, mt, :], V[:, mt, hd * 512:(hd + 1) * 512], mt == 0, mt == 1, [PT_, V])
            P.op(ACT, lambda e: e.activation(ot_[:, :], po[:, 0:512], AF.Copy, scale=st_[:, 3:4]),
                 reads=[po, st_], writes=[ot_])

        def stC(ui_):
            tt, hd = units[ui_]
            ot_ = otok[ui_ % 3]
            ts = slice(tt * 128, (tt + 1) * 128)
            pt2 = npb()
            for i in range(4):
                tp(pt2, pt2[:, i * 128:(i + 1) * 128], ot_[:, i * 128:(i + 1) * 128], ident_b(), [ot_, cbf])
            for i in range(4):
                j = 4 * hd + i
                if hd % 2:
                    P.op(ACT, lambda e, i=i, j=j: e.activation(oT[j][:, ts], pt2[:, i * 128:(i + 1) * 128], AF.Copy), reads=[pt2], writes=[oT[j]])
                else:
                    P.op(DVE, lambda e, i=i, j=j: e.tensor_copy(oT[j][:, ts], pt2[:, i * 128:(i + 1) * 128]), reads=[pt2], writes=[oT[j]])

        NU = len(units)
        for it in range(NU + 5):
            if it < NU:
                stA(it)
            if 0 <= it - 2 < NU:
                stB(it - 2)
            if 0 <= it - 3 < NU:
                stB2(it - 3)
            if 0 <= it - 5 < NU:
                stC(it - 5)
        Prog.cur_tag = "xa.wo"
        gemm_fm(w["wo"], l * D, KC, 0, D, lambda k: oT[k][:, :], oT, T, resid_add(1.0))
        P.release(m)

    P.dma(SP, prm, [(prm[:, :], prm_d[:, :])])
    P.dma(SP, cst, [(cst[:, :], cst_d[:, :])])
    P.op(DVE, lambda e: e.tensor_copy(cbf[:, :], cst[:, 0:384]), reads=[cst], writes=[cbf])
    oc = [0]
    for seq in range(NSEQ):
        for hp in range(NPASS):
            first = (hp == 0)
            tok0 = seq * S + hp * T
            passno[0] = seq * NPASS + hp
            blk[0] = 0
            if passno[0] == 1:
                wi = Ins(POOL, None)
                wi.deps = [Ev(t_.dsem, None, P.dsems[t_.dsem][1]) for t_ in wst if t_.dsem is not None]
                P.q[POOL].append(wi)
            Prog.cur_tag = "io.in"
            m = P.mark()
            stg = [P.sb("stg%d" % i, [128, D], F32) for i in range(2)]
            for tt in range(NTT):
                st_ = stg[tt % 2]
                r0 = tok0 + tt * 128
                P.dma(SP, st_, [(st_[:, :], x_d[r0:r0 + 128, :])])
                for k0 in range(0, KC, 4):
                    ps = nps()
                    for i in range(4):
                        tp(ps, ps[:, i * 128:(i + 1) * 128], st_[:, (k0 + i) * 128:(k0 + i + 1) * 128], ident_f(), [st_, cst])
                    for i in range(4):
                        k = k0 + i
                        if (k0 // 4) % 2:
                            P.op(ACT, lambda e, ps=ps, i=i, k=k, tt=tt: e.activation(h[k][:, tt * 128:(tt + 1) * 128], ps[:, i * 128:(i + 1) * 128], AF.Copy),
                                 reads=[ps], writes=[h[k]])
                        else:
                            P.op(DVE, lambda e, ps=ps, i=i, k=k, tt=tt: e.tensor_copy(h[k][:, tt * 128:(tt + 1) * 128], ps[:, i * 128:(i + 1) * 128]),
                                 reads=[ps], writes=[h[k]])
            P.release(m)
            for l in range(L):
                ffn(l, "ffn1", l * PL + 0)
                mixer(l, first, tok0)
                xattn(l, seq, first)
                ffn(l, "ffn2", l * PL + 64)
            Prog.cur_tag = "io.out"
            rmsnorm(L * PL, None, inplace=True)
            m = P.mark()
            ost = [P.sb("ost%d" % i, [128, D], F32) for i in range(2)]
            for tt in range(NTT):
                o_ = ost[tt % 2]
                for k0 in range(0, KC, 4):
                    ps = nps()
                    for i in range(4):
                        tp(ps, ps[:, i * 128:(i + 1) * 128], h[k0 + i][:, tt * 128:(tt + 1) * 128], ident_f(), [h[k0 + i], cst])
                    eng = ev_eng()
                    if eng == ACT:
                        P.op(ACT, lambda e, ps=ps, o_=o_, k0=k0: e.activation(o_[:, k0 * 128:(k0 + 4) * 128], ps[:, 0:512], AF.Copy), reads=[ps], writes=[o_])
                    else:
                        P.op(DVE, lambda e, ps=ps, o_=o_, k0=k0: e.tensor_copy(o_[:, k0 * 128:(k0 + 4) * 128], ps[:, 0:512]), reads=[ps], writes=[o_])
                r0 = tok0 + tt * 128
                od = out_tl[oc[0] % 2]
                oc[0] += 1
                P.dma(SP, od, [(out_d[r0:r0 + 128, :], o_[:, :])], src=o_)
            P.release(m)
    P.barrier(engs=(PE, ACT, DVE, POOL, SP), final=True)
    P.emit()
    return nc, P


def _consts():
    c = np.zeros((128, 448), np.float32)
    c[:, 0:128] = np.eye(128, dtype=np.float32)
    c[:, 128:256] = np.triu(np.ones((128, 128), np.float32))
    c[:, 256:384] = 1.0
    for g, wsz in enumerate((2, 4, 8, 16)):
        t = np.arange(16)
        c[:, 384 + g * 16:384 + (g + 1) * 16] = (1.0 / np.minimum(t + 1, wsz)).astype(np.float32)[None, :]
    return c


def _params(inp, L):
    prm = np.zeros((128, L * PL + 16), np.float32)

    def fm(v):
        return np.ascontiguousarray(np.asarray(v, np.float32).reshape(-1, 128).T)

    for l in range(L):
        b = l * PL
        prm[:, b + 0:b + 16] = fm(inp["ffn1_norm"][l])
        prm[:, b + 16:b + 32] = fm(inp["mix_norm"][l])
        prm[:, b + 32:b + 48] = fm(inp["xattn_norm"][l])
        prm[:, b + 48:b + 64] = fm(inp["mem_norm"][l])
        prm[:, b + 64:b + 80] = fm(inp["ffn2_norm"][l])
        cw = np.asarray(inp["qk_conv"][l], np.float32)
        prm[:, b + 80:b + 144] = cw.reshape(4, 16, 128).transpose(2, 1, 0).reshape(128, 64)
        prm[:, b + 144:b + 152] = fm(inp["pool_scale"][l])
        prm[:, b + 152:b + 160] = fm(inp["head_norm"][l])
        prm[:, b + 160:b + 168] = np.asarray(inp["gate_bias"][l], np.float32)[None, :]
    prm[:, L * PL:L * PL + 16] = fm(inp["final_norm"])
    return prm


_CACHE = {}


def run(inp, NSEQ, S, T, DEPTH, ncores, trace=False):
    key = (NSEQ, S, T, DEPTH)
    if key not in _CACHE:
        _CACHE[key] = build(NSEQ, S, T, DEPTH)
    nc, P = _CACHE[key]
    L = DEPTH
    f32 = lambda a: np.ascontiguousarray(np.asarray(a, np.float32))
    shared = {
        "prm": _params(inp, L), "cst": _consts(),
        "w_in": f32(inp["w_in"]).reshape(L * D, DIN), "w_out": f32(inp["w_out"]).reshape(L * D, D),
        "pool_w": f32(inp["pool_w"]).reshape(L * 1024, 256),
        "xattn_wq": f32(inp["xattn_wq"]).reshape(L * D, D), "xattn_wkv": f32(inp["xattn_wkv"]).reshape(L * D, 2 * D),
        "xattn_wo": f32(inp["xattn_wo"]).reshape(L * D, D),
    }
    for f in ("ffn1", "ffn2"):
        shared[f + "_w_gate"] = f32(inp[f + "_w_gate"]).reshape(L * D, FF)
        shared[f + "_w_up"] = f32(inp[f + "_w_up"]).reshape(L * D, FF)
        shared[f + "_w_down"] = f32(inp[f + "_w_down"]).reshape(L * FF, D)
    x = f32(inp["x"])
    mem = f32(inp["mem"])
    in_maps = []
    for c in range(ncores):
        d = dict(shared)
        d["x"] = x[c * NSEQ:(c + 1) * NSEQ].reshape(NSEQ * S, D)
        d["mem"] = mem[c * NSEQ:(c + 1) * NSEQ].reshape(NSEQ * NMEM, D)
        in_maps.append(d)
    res = run_bass_kernel_spmd(nc, in_maps, core_ids=list(range(ncores)), **({"trace": True} if trace else {}))
    out = np.stack([res.results[c]["out"].reshape(NSEQ, S, D) for c in range(ncores)], 0).reshape(ncores * NSEQ, S, D)
    return out.astype(np.float32), res


def kernel(**inputs):
    out, _ = run(inputs, NSEQ=2, S=2048, T=512, DEPTH=4, ncores=8)
    return out
```

```python
import contextlib
import numpy as np
import concourse.bass as bass
import concourse.mybir as mybir
from concourse.bass_utils import run_bass_kernel_spmd

F32 = mybir.dt.float32
BF16 = mybir.dt.bfloat16
ALU = mybir.AluOpType
AF = mybir.ActivationFunctionType

PE, ACT, DVE, POOL, SP = "pe", "act", "dve", "pool", "sp"
ENGS = (PE, ACT, DVE, POOL, SP)

D = 2048
KC = 16
FF = 5632
DIN = 5128
NMEM = 256
EPS = 1e-6
PL = 168
SB_BASE = 16640
SB_END = 229376
STRICT = True


class Ev:
    __slots__ = ("key", "ins", "val")

    def __init__(self, key, ins=None, val=None):
        self.key, self.ins, self.val = key, ins, val


class Ins:
    __slots__ = ("eng", "fn", "deps", "need_inc", "semval", "dma_sem", "tag")

    def __init__(self, eng, fn):
        self.eng, self.fn = eng, fn
        self.tag = Prog.cur_tag
        self.deps = []
        self.need_inc = False
        self.semval = None
        self.dma_sem = None


class Tl:
    __slots__ = ("t", "name", "lw", "rd", "dsem", "inh")

    def __init__(self, t, name):
        self.t, self.name = t, name
        self.lw = None
        self.rd = {}
        self.dsem = None
        self.inh = []

    def __getitem__(self, idx):
        return self.t[idx]


class Prog:
    cur_tag = ""

    def __init__(self, nc):
        self.nc = nc
        self.q = {e: [] for e in ENGS}
        self.es = contextlib.ExitStack()
        self.dsems = {}
        self.nd = 0
        self.last = {e: None for e in ENGS}
        self.sp_ptr = SB_BASE
        self.nalloc = 0
        self.live = []
        self.dead = []

    def sb(self, name, shape, dt):
        n = 1
        for s in shape[1:]:
            n *= s
        nbytes = n * (4 if dt == F32 else 2)
        nbytes = (nbytes + 63) // 64 * 64
        off = self.sp_ptr
        assert off + nbytes <= SB_END, "SBUF overflow at %s (%d)" % (name, off + nbytes)
        self.sp_ptr += nbytes
        self.nalloc += 1
        t = self.nc.alloc_sbuf_tensor_at("%s_%d" % (name, self.nalloc), list(shape), dt, offset=off)
        tl = Tl(t, name)
        end = off + nbytes
        keep = []
        for (o, e_, evs) in self.dead:
            if o < end and off < e_:
                tl.inh.extend(evs)
                if off <= o and e_ <= end:
                    continue
            keep.append((o, e_, evs))
        self.dead = keep
        self.live.append((off, end, tl))
        return tl

    def mark(self):
        return self.sp_ptr

    def release(self, m):
        keep = []
        for (o, e_, tl) in self.live:
            if o >= m:
                evs = list(tl.inh) + list(tl.rd.values()) + ([tl.lw] if tl.lw is not None else [])
                if evs:
                    self.dead.append((o, e_, evs))
            else:
                keep.append((o, e_, tl))
        self.live = keep
        self.sp_ptr = m

    def ps(self, name, shape, dt=F32):
        t = self.es.enter_context(self.nc.psum_tensor(name, list(shape), dt))
        return Tl(t, name)

    def _dsem(self, tl):
        if tl.dsem is None:
            key = "d_" + tl.name
            if key not in self.dsems:
                h = self.es.enter_context(self.nc.semaphore(key))
                self.dsems[key] = [h, 0]
            tl.dsem = key
        return tl.dsem

    def _deps(self, ins, reads, writes):
        eng = ins.eng
        deps = []
        for tl in reads:
            if tl.lw is not None:
                deps.append(tl.lw)
        for tl in writes:
            if tl.lw is not None:
                deps.append(tl.lw)
            for k, ev in tl.rd.items():
                if k == eng and not STRICT:
                    continue
                deps.append(ev)
        for tl in list(reads) + list(writes):
            if tl.inh:
                for ev in tl.inh:
                    if ev.key != eng or STRICT:
                        deps.append(ev)
        for tl in writes:
            tl.inh = []
        out = []
        for ev in deps:
            if ev.ins is ins:
                continue
            if ev.key == PE and eng == PE:
                continue
            out.append(ev)
            if ev.ins is not None:
                ev.ins.need_inc = True
        ins.deps = out

    def op(self, eng, fn, reads=(), writes=()):
        ins = Ins(eng, fn)
        self._deps(ins, reads, writes)
        ev = Ev(eng, ins)
        for tl in reads:
            tl.rd[eng] = ev
        for tl in writes:
            tl.lw = ev
            tl.rd = {}
        self.q[eng].append(ins)
        self.last[eng] = ev
        return ins

    def dma(self, queue, dst, pairs, src=None):
        key = self._dsem(dst)
        rec = self.dsems[key]

        def fn(e, pairs=pairs, h=rec[0]):
            r = None
            for (o, i) in pairs:
                r = e.dma_start(out=o, in_=i).then_inc(h, 16)
            return r

        ins = Ins(queue, fn)
        ins.dma_sem = key
        rds = [src] if src is not None else []
        self._deps(ins, rds, [dst])
        if rec[1] > 0:
            ins.deps.append(Ev(key, None, rec[1]))
        rec[1] += 16 * len(pairs)
        ev = Ev(key, None, rec[1])
        for tl in rds:
            tl.rd[key] = ev
        dst.lw = ev
        dst.rd = {}
        self.q[queue].append(ins)
        return ins

    def barrier(self, engs=(PE, ACT, DVE, SP), final=False):
        evs = [self.last[e] for e in engs if self.last[e] is not None]
        devs = [Ev(k, None, rec[1]) for k, rec in self.dsems.items() if rec[1] > 0 and (final or not k.startswith("d_ring"))]
        for e in engs:
            ins = Ins(e, None)
            ins.deps = [ev for ev in evs if ev.key != e] + devs
            for ev in ins.deps:
                if ev.ins is not None:
                    ev.ins.need_inc = True
            self.q[e].append(ins)

    def emit(self):
        nc = self.nc
        sems = {}
        for e in ENGS:
            sems[e] = self.es.enter_context(nc.semaphore("eng_" + e))
            c = 0
            for ins in self.q[e]:
                if ins.dma_sem is None and ins.need_inc:
                    c += 1
                    ins.semval = c
        self.stats = {e: [len(self.q[e]), 0] for e in ENGS}

        def handle(key):
            return sems[key] if key in sems else self.dsems[key][0]

        self.emitted = {e: [] for e in ENGS}

        def run(eng_name, eobj):
            seen = {}
            em = self.emitted[eng_name]
            for ins in self.q[eng_name]:
                need = {}
                for ev in ins.deps:
                    v = ev.val if ev.ins is None else ev.ins.semval
                    if seen.get(ev.key, 0) >= v:
                        continue
                    if need.get(ev.key, 0) < v:
                        need[ev.key] = v
                for k, v in need.items():
                    eobj.wait_ge(handle(k), v)
                    em.append(("W", ins.tag, k))
                    seen[k] = v
                    self.stats[eng_name][1] += 1
                if ins.fn is None:
                    continue
                r = ins.fn(eobj)
                em.append(("I", ins.tag, ""))
                if ins.dma_sem is None and ins.need_inc:
                    r.then_inc(sems[eng_name], 1)

        with nc.Block() as block:
            @block.tensor
            def _(e):
                run(PE, e)

            @block.scalar
            def _(e):
                run(ACT, e)

            @block.vector
            def _(e):
                run(DVE, e)

            @block.gpsimd
            def _(e):
                run(POOL, e)

            @block.sync
            def _(e):
                run(SP, e)


def build(NSEQ, S, T, DEPTH):
    nc = bass.Bass("TRN2", target_bir_lowering=False)
    P = Prog(nc)
    NTT = T // 128
    NPASS = S // T
    L = DEPTH

    def din(name, shape):
        return nc.dram_tensor(name, list(shape), F32, kind="ExternalInput").ap()

    x_d = din("x", [NSEQ * S, D])
    mem_d = din("mem", [NSEQ * NMEM, D])
    w = {}
    for f in ("ffn1", "ffn2"):
        w[f + "_g"] = din(f + "_w_gate", [L * D, FF])
        w[f + "_u"] = din(f + "_w_up", [L * D, FF])
        w[f + "_d"] = din(f + "_w_down", [L * FF, D])
    w["in"] = din("w_in", [L * D, DIN])
    w["out"] = din("w_out", [L * D, D])
    w["pool"] = din("pool_w", [L * 1024, 256])
    w["wq"] = din("xattn_wq", [L * D, D])
    w["wkv"] = din("xattn_wkv", [L * D, 2 * D])
    w["wo"] = din("xattn_wo", [L * D, D])
    NPRM = L * PL + 16
    prm_d = din("prm", [128, NPRM])
    cst_d = din("cst", [128, 448])
    out_d = nc.dram_tensor("out", [NSEQ * S, D], F32, kind="ExternalOutput").ap()
    out_tl = [Tl(out_d, "out%d" % i) for i in range(2)]

    prm = P.sb("prm", [128, NPRM], F32)
    cst = P.sb("cst", [128, 448], F32)
    ident_f = lambda: cst[:, 0:128]
    triu_f = lambda: cst[:, 128:256]
    ones_f = lambda: cst[:, 256:384]
    cbf = P.sb("cbf", [128, 384], BF16)
    ident_b = lambda: cbf[:, 0:128]
    mask_b = lambda: cbf[:, 128:256]
    ones_b = lambda: cbf[:, 256:384]
    h = [P.sb("h%d" % k, [128, T], F32) for k in range(KC)]
    Cst = [[P.sb("Cst%d_%d" % (l, hd), [128, 2, 257], F32) for hd in range(4)] for l in range(L)]
    ztail = [[P.sb("zt%d_%d" % (l, c), [128, 3], F32) for c in range(16)] for l in range(L)]
    ptail = [[P.sb("pt%d_%d" % (l, c), [128, 16], F32) for c in range(8)] for l in range(L)]
    NR = 7
    ring = [P.sb("ring%d" % i, [128, 4096], BF16) for i in range(NR)]
    rstate = [0]
    NBLK = L * 222
    wscr = [nc.dram_tensor("wscr%d" % l_, [222, 128, 4096], BF16, kind="Internal").ap() for l_ in range(L)]
    wst = [Tl(wscr[0], "wst%d" % i) for i in range(NR)]
    blk = [0]
    passno = [0]
    kvscr = nc.dram_tensor("kvscr", [L * 2, 128, 4096], BF16, kind="Internal").ap()
    kvs_k = [Tl(kvscr, "kvk%d" % l_) for l_ in range(L)]
    kvs_v = [Tl(kvscr, "kvv%d" % l_) for l_ in range(L)]
    psf = [P.ps("psf%d" % i, [128, 512], F32) for i in range(6)]
    psb = [P.ps("psb%d" % i, [128, 512], BF16) for i in range(2)]
    pstate = [0, 0]

    def nps():
        pstate[0] += 1
        return psf[pstate[0] % 6]

    def npb():
        pstate[1] += 1
        return psb[pstate[1] % 2]

    rr = [0]

    def ev_eng():
        rr[0] += 1
        return ACT if rr[0] % 2 else DVE

    def mm(ps, out_ap, lhsT, rhs, start, stop, reads):
        P.op(PE, lambda e: e.matmul(out_ap, lhsT, rhs, start=start, stop=stop), reads=reads, writes=[ps])

    def tp(ps, out_ap, in_ap, ident, reads):
        P.op(PE, lambda e: e.transpose(out_ap, in_ap, ident), reads=reads, writes=[ps])

    def wload(wd, r0, nk, c0, bw):
        si = rstate[0] % NR
        sl = ring[si]
        rstate[0] += 1
        b = blk[0]
        blk[0] += 1
        scr = wscr[b // 222][b % 222][:, 0:nk * bw]
        sv = sl[:, 0:nk * bw].rearrange("p (k c) -> p k c", k=nk)
        if passno[0] == 0:
            src = wd[r0:r0 + nk * 128, c0:c0 + bw].rearrange("(r p) c -> p r c", p=128)
            P.dma(POOL, sl, [(sv, src)])
            if NSEQ * NPASS > 1:
                P.dma(SP, wst[si], [(scr, sl[:, 0:nk * bw])], src=sl)
        else:
            P.dma(POOL, sl, [(sl[:, 0:nk * bw], scr)])
        return sl, sv

    def gemm_fm(wd, r0, nk, c0, ncols, rhs, rhs_tl, n, evac):
        for cb in range(0, ncols, 256):
            bw = min(256, ncols - cb)
            sl, sv = wload(wd, r0, nk, c0 + cb, bw)
            for dj in range(bw // 128):
                ps = nps()
                for k in range(nk):
                    mm(ps, ps[:, 0:n], sv[:, k, dj * 128:(dj + 1) * 128], rhs(k), k == 0, k == nk - 1,
                       [sl, rhs_tl[k]])
                evac(cb // 128 + dj, ps)

    def gemm_tm(wd, r0, nk, c0, ncols, lhs, lhs_tl, ntile, evac):
        for cb in range(0, ncols, 256):
            bw = min(256, ncols - cb)
            sl, sv = wload(wd, r0, nk, c0 + cb, bw)
            for tt in range(ntile):
                ps = nps()
                for k in range(nk):
                    mm(ps, ps[:, 0:bw], lhs(k, tt), sv[:, k, 0:bw], k == 0, k == nk - 1, [sl, lhs_tl[k]])
                evac(cb, bw, tt, ps)

    def rsqrt_chain(buf, ap, ap2):
        P.op(ACT, lambda e: e.activation(ap, ap2, AF.Sqrt), reads=[buf], writes=[buf])
        P.op(DVE, lambda e: e.reciprocal(ap, ap2), reads=[buf], writes=[buf])

    def rmsnorm(gcol, xn, inplace=False):
        old_tag = Prog.cur_tag
        Prog.cur_tag = old_tag + ".norm"
        m = P.mark()
        sq = [P.sb("sq%d" % i, [128, T], BF16) for i in range(3)]
        rs = P.sb("rs", [128, T], F32)
        ps = nps()
        for k in range(KC):
            s_ = sq[k % 3]
            P.op(ACT, lambda e, s_=s_, k=k: e.activation(s_[:, :], h[k][:, :], AF.Square), reads=[h[k]], writes=[s_])
            mm(ps, ps[:, 0:T], ones_b(), s_[:, :], k == 0, k == KC - 1, [s_, cbf])
        P.op(ACT, lambda e: e.activation(rs[:, :], ps[:, 0:T], AF.Sqrt, scale=1.0 / D, bias=EPS), reads=[ps], writes=[rs])
        P.op(DVE, lambda e: e.reciprocal(rs[:, :], rs[:, :]), reads=[rs], writes=[rs])
        for k in range(KC):
            dst = h[k] if inplace else xn[k]
            P.op(DVE, lambda e, k=k, dst=dst: e.scalar_tensor_tensor(
                dst[:, :], h[k][:, :], prm[:, gcol + k:gcol + k + 1], rs[:, :], ALU.mult, ALU.mult),
                reads=[h[k], rs, prm], writes=[dst])
        P.release(m)
        Prog.cur_tag = old_tag

    def resid_add(scale):
        def evac(j, ps):
            P.op(DVE, lambda e: e.scalar_tensor_tensor(h[j][:, :], ps[:, 0:T], scale, h[j][:, :], ALU.mult, ALU.add),
                 reads=[ps, h[j]], writes=[h[j]])
        return evac

    def ffn(l, name, gcol):
        Prog.cur_tag = "ffn"
        m = P.mark()
        xn = [P.sb("xn%d" % k, [128, T], BF16) for k in range(KC)]
        hT = [P.sb("hT%d" % k, [128, T], BF16) for k in range(FF // 128)]
        rmsnorm(gcol, xn)
        Prog.cur_tag = "ffn.gu"
        m2 = P.mark()
        stmp = [P.sb("stmp%d" % i, [128, T], F32) for i in range(3)]
        sc = [0]
        wg, wu, wd = w[name + "_g"], w[name + "_u"], w[name + "_d"]
        for cb in range(0, FF, 256):
            tmps = {}

            def ev_g(j, ps, tmps=tmps):
                t_ = stmp[sc[0] % 3]
                sc[0] += 1
                tmps[j] = t_
                P.op(ACT, lambda e: e.activation(t_[:, :], ps[:, 0:T], AF.Silu), reads=[ps], writes=[t_])

            def ev_u(j, ps, tmps=tmps):
                t_ = tmps[j]
                P.op(DVE, lambda e: e.tensor_tensor(hT[j][:, :], t_[:, :], ps[:, 0:T], ALU.mult), reads=[ps, t_], writes=[hT[j]])

            base = cb // 128
            gemm_fm(wg, l * D, KC, cb, 256, lambda k: xn[k][:, :], xn, T, lambda j, ps: ev_g(base + j, ps))
            gemm_fm(wu, l * D, KC, cb, 256, lambda k: xn[k][:, :], xn, T, lambda j, ps: ev_u(base + j, ps))
        P.release(m2)
        Prog.cur_tag = "ffn.down"
        for g0 in range(0, 44, 11):
            gemm_fm(wd, l * FF + g0 * 128, 11, 0, D, lambda k, g0=g0: hT[g0 + k][:, :], hT[g0:g0 + 11], T, resid_add(0.5))
        P.release(m)

    def mixer(l, first, tok0):
        pb = l * PL
        Prog.cur_tag = "mix"
        m = P.mark()
        xn = [P.sb("xn%d" % k, [128, T], BF16) for k in range(KC)]
        rmsnorm(pb + 16, xn)
        Prog.cur_tag = "mix.pool"
        xr = lambda k: xn[k][:, :]
        m1 = P.mark()
        dT = [P.sb("dT%d" % c, [128, T], BF16) for c in range(8)]
        ypT = [P.sb("ypT%d" % c, [128, T], BF16) for c in range(8)]
        pbuf = [P.sb("pbuf%d" % i, [128, 16 + T], F32) for i in range(2)]
        pa = [P.sb("pa%d" % i, [128, 16 + T], F32) for i in range(2)]
        pc_ = [0]

        def ev_p(c, ps):
            X = pbuf[pc_[0] % 2]
            pc_[0] += 1
            wlog = c // 2 + 1
            wsz = 1 << wlog
            if first:
                P.op(DVE, lambda e: e.memset(X[:, 0:16], 0.0), writes=[X])
            else:
                P.op(DVE, lambda e: e.tensor_copy(X[:, 0:16], ptail[l][c][:, :]), reads=[ptail[l][c]], writes=[X])
            P.op(ACT, lambda e: e.activation(X[:, 16:16 + T], ps[:, 0:T], AF.Copy), reads=[ps], writes=[X])
            P.op(ACT, lambda e: e.activation(ptail[l][c][:, :], X[:, T:T + 16], AF.Copy), reads=[X], writes=[ptail[l][c]])
            src, lo = X, 0
            for i in range(wlog):
                sh = 1 << i
                dst = pa[i % 2]
                nlo = lo + sh
                P.op(DVE, lambda e, src=src, dst=dst, nlo=nlo, sh=sh: e.tensor_tensor(
                    dst[:, nlo:16 + T], src[:, nlo:16 + T], src[:, nlo - sh:16 + T - sh], ALU.add),
                    reads=[src], writes=[dst])
                src, lo = dst, nlo
            A = src
            P.op(DVE, lambda e: e.scalar_tensor_tensor(dT[c][:, :], A[:, 16:16 + T], 1.0 / wsz, X[:, 16:16 + T],
                                                       ALU.mult, ALU.subtract), reads=[A, X], writes=[dT[c]])
            if first:
                g = c // 2
                P.op(DVE, lambda e: e.tensor_tensor(A[:, 16:32], A[:, 16:32], cst[:, 384 + g * 16:384 + (g + 1) * 16], ALU.mult),
                     reads=[A, cst], writes=[A])
                P.op(DVE, lambda e: e.tensor_tensor(dT[c][:, 0:16], A[:, 16:32], X[:, 16:32], ALU.subtract),
                     reads=[A, X], writes=[dT[c]])

        gemm_fm(w["in"], l * D, KC, 0, 1024, xr, xn, T, ev_p)
        slp, svp = wload(w["pool"], l * 1024, 8, 0, 256)
        for g in range(4):
            for dc in range(2):
                ps = nps()
                for cc in range(2):
                    mm(ps, ps[:, 0:T], svp[:, 2 * g + cc, dc * 128:(dc + 1) * 128], dT[2 * g + cc][:, :], cc == 0, cc == 1,
                       [slp, dT[2 * g + cc]])
                j = 2 * g + dc
                P.op(ACT, lambda e, j=j, ps=ps: e.activation(ypT[j][:, :], ps[:, 0:T], AF.Copy,
                                                             scale=prm[:, pb + 144 + j:pb + 145 + j]),
                     reads=[ps, prm], writes=[ypT[j]])
        gemm_fm(w["out"], l * D, 8, 0, D, lambda k: ypT[k][:, :], ypT, T, resid_add(1.0))
        P.release(m1)
        Prog.cur_tag = "mix.qk"
        qkT = [P.sb("qkT%d" % c, [128, T], BF16) for c in range(16)]
        m2 = P.mark()
        zb = [P.sb("zb%d" % i, [128, 3 + T], F32) for i in range(2)]
        za = [P.sb("za%d" % i, [128, T], F32) for i in range(2)]
        zc = [0]

        def ev_qk(c, ps):
            Z = zb[zc[0] % 2]
            A = za[zc[0] % 2]
            zc[0] += 1
            if first:
                P.op(DVE, lambda e: e.memset(Z[:, 0:3], 0.0), writes=[Z])
            else:
                P.op(DVE, lambda e: e.tensor_copy(Z[:, 0:3], ztail[l][c][:, :]), reads=[ztail[l][c]], writes=[Z])
            P.op(ACT, lambda e: e.activation(Z[:, 3:3 + T], ps[:, 0:T], AF.Copy), reads=[ps], writes=[Z])
            P.op(ACT, lambda e: e.activation(ztail[l][c][:, :], Z[:, T:T + 3], AF.Copy), reads=[Z], writes=[ztail[l][c]])
            cw = pb + 80 + c * 4
            P.op(DVE, lambda e: e.tensor_scalar(A[:, :], Z[:, 0:T], prm[:, cw:cw + 1], 0.0, ALU.mult, ALU.add), reads=[Z, prm], writes=[A])
            for j in range(1, 4):
                P.op(DVE, lambda e, j=j: e.scalar_tensor_tensor(A[:, :], Z[:, j:j + T], prm[:, cw + j:cw + j + 1], A[:, :],
                                                                ALU.mult, ALU.add), reads=[Z, A, prm], writes=[A])
            P.op(ACT, lambda e: e.activation(qkT[c][:, :], A[:, :], AF.Silu), reads=[A], writes=[qkT[c]])

        gemm_fm(w["in"], l * D, KC, 1024, 2048, xr, xn, T, ev_qk)
        P.release(m2)
        Prog.cur_tag = "mix.vog"
        vaug = [P.sb("vaug%d" % tt, [128, 4, 257], BF16) for tt in range(NTT)]
        G = [P.sb("G%d" % tt, [128, 1024], BF16) for tt in range(NTT)]
        gsc = [P.sb("gsc%d" % tt, [128, 12], F32) for tt in range(NTT)]
        gtmp = [P.sb("gtmp%d" % tt, [128, 16], F32) for tt in range(NTT)]
        for tt in range(NTT):
            P.op(DVE, lambda e, tt=tt: e.memset(vaug[tt][:, :, 256:257], 1.0), writes=[vaug[tt]])
        xl = lambda k, tt: xn[k][:, tt * 128:(tt + 1) * 128]

        def ev_v(cb, bw, tt, ps):
            hd = cb // 256
            eng = ev_eng()
            if eng == ACT:
                P.op(ACT, lambda e: e.activation(vaug[tt][:, hd, 0:256], ps[:, 0:256], AF.Copy), reads=[ps], writes=[vaug[tt]])
            else:
                P.op(DVE, lambda e: e.tensor_copy(vaug[tt][:, hd, 0:256], ps[:, 0:256]), reads=[ps], writes=[vaug[tt]])

        def ev_og(cb, bw, tt, ps):
            P.op(ACT, lambda e: e.activation(G[tt][:, cb:cb + bw], ps[:, 0:bw], AF.Sigmoid), reads=[ps], writes=[G[tt]])

        def ev_gate(cb, bw, tt, ps):
            gt, gs = gtmp[tt], gsc[tt]
            gb = pb + 160
            P.op(DVE, lambda e: e.tensor_tensor(gt[:, 0:8], ps[:, 0:8], prm[:, gb:gb + 8], ALU.add), reads=[ps, prm], writes=[gt])
            P.op(ACT, lambda e: e.activation(gt[:, 8:12], gt[:, 4:8], AF.Exp, scale=-1.0), reads=[gt], writes=[gt])
            P.op(ACT, lambda e: e.activation(gt[:, 8:12], gt[:, 8:12], AF.Ln, bias=1.0), reads=[gt], writes=[gt])
            gate_pending.append((gt, gs))

        def gate_finish(gt, gs):
            pg = nps()
            mm(pg, pg[:, 0:4], triu_f(), gt[:, 8:12], True, True, [cst, gt])
            mm(pg, pg[:, 4:8], ones_f(), gt[:, 8:12], True, True, [cst, gt])
            P.op(DVE, lambda e: e.tensor_tensor(gt[:, 12:16], gt[:, 0:4], pg[:, 0:4], ALU.add), reads=[gt, pg], writes=[gt])
            P.op(ACT, lambda e: e.activation(gs[:, 0:4], gt[:, 12:16], AF.Exp), reads=[gt], writes=[gs])
            P.op(ACT, lambda e: e.activation(gs[:, 4:12], pg[:, 0:8], AF.Exp, scale=-1.0), reads=[pg], writes=[gs])

        gate_pending = []
        gemm_tm(w["in"], l * D, KC, 5120, 8, xl, xn, NTT, ev_gate)
        gemm_tm(w["in"], l * D, KC, 3072, 1024, xl, xn, NTT, ev_v)
        for (gt_, gs_) in gate_pending:
            gate_finish(gt_, gs_)
        gemm_tm(w["in"], l * D, KC, 4096, 1024, xl, xn, NTT, ev_og)
        Prog.cur_tag = "mix.mlstm"
        ymT = [P.sb("ymT%d" % c, [128, T], BF16) for c in range(8)]
        Cb = [P.sb("Cb%d" % hd, [128, 2, 257], BF16) for hd in range(4)]
        Wp = [P.sb("Wp%d" % i, [128, 128], BF16) for i in range(2)]
        kc = [P.sb("kc%d" % i, [128, 256], BF16) for i in range(2)]
        ytok = [P.sb("ytok%d" % i, [128, 256], BF16) for i in range(2)]
        junk = P.sb("junk", [128, 256], F32)
        scs = [P.sb("scs%d" % i, [128, 40], F32) for i in range(2)]
        if first:
            for hd in range(4):
                P.op(DVE, lambda e, hd=hd: e.memset(Cst[l][hd][:, :, :], 0.0), writes=[Cst[l][hd]])
        for hd in range(4):
            P.op(ACT, lambda e, hd=hd: e.activation(Cb[hd][:, :, :], Cst[l][hd][:, :, :], AF.Copy), reads=[Cst[l][hd]], writes=[Cb[hd]])
        def stA(tt, hd):
            ts = slice(tt * 128, (tt + 1) * 128)
            gs = gsc[tt]
            pS = psf[4]
            for i in range(2):
                mm(pS, pS[:, 0:128], qkT[8 + 2 * hd + i][:, ts], qkT[2 * hd + i][:, ts], i == 0, i == 1,
                   [qkT[8 + 2 * hd + i], qkT[2 * hd + i]])
            W_ = Wp[hd % 2]
            P.op(DVE, lambda e: e.scalar_tensor_tensor(W_[:, :], pS[:, 0:128], gs[:, hd:hd + 1], mask_b(), ALU.mult, ALU.mult),
                 reads=[pS, gs, cbf], writes=[W_])
            pk = npb()
            for i in range(2):
                tp(pk, pk[:, i * 128:(i + 1) * 128], qkT[8 + 2 * hd + i][:, ts], ident_b(), [qkT[8 + 2 * hd + i], cbf])
            kc_ = kc[hd % 2]
            P.op(ACT, lambda e: e.activation(kc_[:, :], pk[:, 0:256], AF.Copy, scale=gs[:, hd:hd + 1]), reads=[pk, gs], writes=[kc_])

        def stB(tt, hd):
            ts = slice(tt * 128, (tt + 1) * 128)
            gs = gsc[tt]
            sc = scs[tt % 2]
            W_ = Wp[hd % 2]
            kc_ = kc[hd % 2]
            pn = psf[hd]
            mm(pn, pn[:, 0:257], W_[:, :], vaug[tt][:, hd, :], True, False, [W_, vaug[tt]])
            for i in range(2):
                mm(pn, pn[:, 0:257], qkT[2 * hd + i][:, ts], Cb[hd][:, i, :], False, i == 1, [qkT[2 * hd + i], Cb[hd]])
            P.op(DVE, lambda e: e.tensor_tensor(sc[:, hd:hd + 1], pn[:, 256:257], gs[:, 4 + hd:5 + hd], ALU.mult),
                 reads=[pn, gs], writes=[sc])
            P.op(ACT, lambda e: e.activation(junk[:, :], pn[:, 0:256], AF.Square, accum_out=sc[:, 4 + hd:5 + hd]),
                 reads=[pn], writes=[junk, sc])
            for i in range(2):
                pd = psf[5 - i]
                mm(pd, pd[:, 0:257], kc_[:, i * 128:(i + 1) * 128], vaug[tt][:, hd, :], True, True, [kc_, vaug[tt]])
                P.op(DVE, lambda e, pd=pd, i=i: e.tensor_tensor(Cst[l][hd][:, i, :], pd[:, 0:257], Cst[l][hd][:, i, :], ALU.add),
                     reads=[pd, Cst[l][hd]], writes=[Cst[l][hd]])

        def fin(tt, a):
            ts = slice(tt * 128, (tt + 1) * 128)
            gs = gsc[tt]
            sc = scs[tt % 2]
            v2 = lambda o: sc[:, o + a:o + a + 2]
            P.op(ACT, lambda e: e.activation(v2(8), v2(0), AF.Abs), reads=[sc], writes=[sc])
            P.op(DVE, lambda e: e.tensor_scalar_max(v2(8), v2(8), 16.0), reads=[sc], writes=[sc])
            P.op(DVE, lambda e: e.reciprocal(v2(12), v2(8)), reads=[sc], writes=[sc])
            P.op(DVE, lambda e: e.tensor_tensor(v2(16), v2(12), gs[:, 4 + a:6 + a], ALU.mult), reads=[sc, gs], writes=[sc])
            P.op(DVE, lambda e: e.tensor_tensor(v2(20), v2(16), v2(16), ALU.mult), reads=[sc], writes=[sc])
            P.op(DVE, lambda e: e.tensor_tensor(v2(24), v2(20), v2(4), ALU.mult), reads=[sc], writes=[sc])
            P.op(DVE, lambda e: e.tensor_scalar(v2(28), v2(24), 1.0 / 256, EPS, ALU.mult, ALU.add), reads=[sc], writes=[sc])
            P.op(ACT, lambda e: e.activation(v2(32), v2(28), AF.Sqrt), reads=[sc], writes=[sc])
            P.op(DVE, lambda e: e.reciprocal(v2(32), v2(32)), reads=[sc], writes=[sc])
            P.op(DVE, lambda e: e.tensor_tensor(v2(36), v2(32), v2(16), ALU.mult), reads=[sc], writes=[sc])
            for hh in (a, a + 1):
                pn_ = psf[hh]
                y_ = ytok[hh % 2]
                P.op(DVE, lambda e, pn_=pn_, y_=y_, hh=hh: e.scalar_tensor_tensor(
                    y_[:, :], pn_[:, 0:256], sc[:, 36 + hh:37 + hh], G[tt][:, hh * 256:(hh + 1) * 256], ALU.mult, ALU.mult),
                    reads=[pn_, sc, G[tt]], writes=[y_])
                P.op(DVE, lambda e, hh=hh: e.tensor_scalar(Cst[l][hh][:, :, :], Cst[l][hh][:, :, :], gs[:, 8 + hh:9 + hh], 0.0, ALU.mult, ALU.add),
                     reads=[Cst[l][hh], gs], writes=[Cst[l][hh]])
                P.op(ACT, lambda e, hh=hh: e.activation(Cb[hh][:, :, :], Cst[l][hh][:, :, :], AF.Copy), reads=[Cst[l][hh]], writes=[Cb[hh]])

        def finP(tt, a):
            ts = slice(tt * 128, (tt + 1) * 128)
            for hh in (a, a + 1):
                y_ = ytok[hh % 2]
                py = npb()
                for i in range(2):
                    tp(py, py[:, i * 128:(i + 1) * 128], y_[:, i * 128:(i + 1) * 128], ident_b(), [y_, cbf])
                for i in range(2):
                    j = 2 * hh + i
                    P.op(ACT, lambda e, py=py, i=i, j=j: e.activation(
                        ymT[j][:, ts], py[:, i * 128:(i + 1) * 128], AF.Copy, scale=prm[:, pb + 152 + j:pb + 153 + j]),
                        reads=[py, prm], writes=[ymT[j]])

        pend = None
        for tt in range(NTT):
            stA(tt, 0)
            stA(tt, 1)
            stB(tt, 0)
            if pend is not None:
                finP(*pend)
            stA(tt, 2)
            stB(tt, 1)
            stA(tt, 3)
            fin(tt, 0)
            stB(tt, 2)
            stB(tt, 3)
            finP(tt, 0)
            fin(tt, 2)
            pend = (tt, 2)
        finP(*pend)
        Prog.cur_tag = "mix.wout2"
        gemm_fm(w["out"], l * D + 1024, 8, 0, D, lambda k: ymT[k][:, :], ymT, T, resid_add(1.0))
        P.release(m)

    def xattn(l, seq, first):
        pb = l * PL
        Prog.cur_tag = "xa"
        m = P.mark()
        xn = [P.sb("xn%d" % k, [128, T], BF16) for k in range(KC)]
        rmsnorm(pb + 32, xn)
        Prog.cur_tag = "xa.memq"
        KT = P.sb("KT", [128, KC, NMEM], BF16)
        V = P.sb("V", [128, 2, D], BF16)
        QT = [P.sb("QT%d" % c, [128, T], BF16) for c in range(KC)]
        qs = float(512 ** -0.5)
        m1 = P.mark()
        mnT = [P.sb("mnT%d" % c, [128, NMEM], BF16) for c in range(KC)] if first else None
        mst = P.sb("mst", [128, D], F32)
        mn = [P.sb("mn%d" % i, [128, D], BF16) for i in range(2)]
        mss = P.sb("mss", [128, 4], F32)
        if first:
            for mt in range(2):
                r0 = seq * NMEM + mt * 128
                P.dma(SP, mst, [(mst[:, :], mem_d[r0:r0 + 128, :])])
                P.op(ACT, lambda e, mt=mt: e.activation(mn[mt][:, :], mst[:, :], AF.Square, accum_out=mss[:, mt:mt + 1]),
                     reads=[mst], writes=[mn[mt], mss])
                P.op(DVE, lambda e, mt=mt: e.tensor_scalar(mss[:, 2 + mt:3 + mt], mss[:, mt:mt + 1], 1.0 / D, EPS, ALU.mult, ALU.add),
                     reads=[mss], writes=[mss])
                rsqrt_chain(mss, mss[:, 2 + mt:3 + mt], mss[:, 2 + mt:3 + mt])
                P.op(DVE, lambda e, mt=mt: e.tensor_scalar(mn[mt][:, :], mst[:, :], mss[:, 2 + mt:3 + mt], 0.0, ALU.mult, ALU.add),
                     reads=[mst, mss], writes=[mn[mt]])

        def ev_q(j, ps):
            P.op(ACT, lambda e: e.activation(QT[j][:, :], ps[:, 0:T], AF.Copy, scale=qs), reads=[ps], writes=[QT[j]])

        gemm_fm(w["wq"], l * D, KC, 0, D, lambda k: xn[k][:, :], xn, T, ev_q)
        if first:
            for mt in range(2):
                for k0 in range(0, KC, 4):
                    pt = npb()
                    for i in range(4):
                        tp(pt, pt[:, i * 128:(i + 1) * 128], mn[mt][:, (k0 + i) * 128:(k0 + i + 1) * 128], ident_b(), [mn[mt], cbf])
                    for i in range(4):
                        k = k0 + i
                        P.op(ACT, lambda e, pt=pt, i=i, k=k, mt=mt: e.activation(
                            mnT[k][:, mt * 128:(mt + 1) * 128], pt[:, i * 128:(i + 1) * 128], AF.Copy,
                            scale=prm[:, pb + 48 + k:pb + 49 + k]), reads=[pt, prm], writes=[mnT[k]])
        def ev_k(j, ps):
            eng = ev_eng()
            if eng == ACT:
                P.op(ACT, lambda e: e.activation(KT[:, j, :], ps[:, 0:NMEM], AF.Copy), reads=[ps], writes=[KT])
            else:
                P.op(DVE, lambda e: e.tensor_copy(KT[:, j, :], ps[:, 0:NMEM]), reads=[ps], writes=[KT])

        def ev_vv(cb, bw, mt, ps):
            eng = ev_eng()
            if eng == ACT:
                P.op(ACT, lambda e: e.activation(V[:, mt, cb:cb + bw], ps[:, 0:bw], AF.Copy), reads=[ps], writes=[V])
            else:
                P.op(DVE, lambda e: e.tensor_copy(V[:, mt, cb:cb + bw], ps[:, 0:bw]), reads=[ps], writes=[V])

        Prog.cur_tag = "xa.kv"
        kview = kvscr[2 * l].rearrange("p (a b) -> p a b", a=KC)
        vview = kvscr[2 * l + 1].rearrange("p (a b) -> p a b", a=2)
        if first:
            gemm_fm(w["wkv"], l * D, KC, 0, D, lambda k: mnT[k][:, :], mnT, NMEM, ev_k)
            gemm_tm(w["wkv"], l * D, KC, D, D, lambda k, mt: mnT[k][:, mt * 128:(mt + 1) * 128], mnT, 2, ev_vv)
            if NPASS > 1:
                P.dma(SP, kvs_k[l], [(kview, KT[:, :, :])], src=KT)
                P.dma(SP, kvs_v[l], [(vview, V[:, :, :])], src=V)
        else:
            blk[0] += 16
            P.dma(SP, KT, [(KT[:, :, :], kview)], src=kvs_k[l])
            P.dma(SP, V, [(V[:, :, :], vview)], src=kvs_v[l])
        P.release(m1)
        Prog.cur_tag = "xa.attn"
        oT = [P.sb("oT%d" % c, [128, T], BF16) for c in range(KC)]
        Pb = [P.sb("Pb%d" % i, [128, NMEM], BF16) for i in range(3)]
        PT = [P.sb("PT%d" % i, [128, 2, 128], BF16) for i in range(2)]
        otok = [P.sb("otok%d" % i, [128, 512], BF16) for i in range(3)]
        ast = [P.sb("ast%d" % i, [128, 4], F32) for i in range(4)]
        units = [(tt, hd) for tt in range(NTT) for hd in range(4)]
        pS_of, po_of = {}, {}

        def stA(ui_):
            tt, hd = units[ui_]
            ts = slice(tt * 128, (tt + 1) * 128)
            pS = nps()
            for i in range(4):
                mm(pS, pS[:, 0:NMEM], QT[4 * hd + i][:, ts], KT[:, 4 * hd + i, :], i == 0, i == 3, [QT[4 * hd + i], KT])
            st_ = ast[ui_ % 4]
            Pb_ = Pb[ui_ % 3]
            P.op(DVE, lambda e: e.reduce_max(st_[:, 0:1], pS[:, 0:NMEM], mybir.AxisListType.X), reads=[pS], writes=[st_])
            P.op(DVE, lambda e: e.tensor_scalar(st_[:, 1:2], st_[:, 0:1], -1.0, 0.0, ALU.mult, ALU.add), reads=[st_], writes=[st_])
            P.op(ACT, lambda e: e.activation(Pb_[:, :], pS[:, 0:NMEM], AF.Exp, bias=st_[:, 1:2], accum_out=st_[:, 2:3]),
                 reads=[pS, st_], writes=[Pb_, st_])
            P.op(DVE, lambda e: e.reciprocal(st_[:, 3:4], st_[:, 2:3]), reads=[st_], writes=[st_])

        def stB(ui_):
            tt, hd = units[ui_]
            Pb_ = Pb[ui_ % 3]
            PT_ = PT[ui_ % 2]
            pt = npb()
            for i in range(2):
                tp(pt, pt[:, i * 128:(i + 1) * 128], Pb_[:, i * 128:(i + 1) * 128], ident_b(), [Pb_, cbf])
            P.op(DVE, lambda e: e.tensor_copy(PT_[:, :, :], pt[:, 0:256].rearrange("p (a b) -> p a b", a=2)),
                 reads=[pt], writes=[PT_])

        def stB2(ui_):
            tt, hd = units[ui_]
            st_ = ast[ui_ % 4]
            PT_ = PT[ui_ % 2]
            ot_ = otok[ui_ % 3]
            po = nps()
            for mt in range(2):
                mm(po, po[:, 0:512], PT_[:, mt, :], V[:, mt, hd * 512:(hd + 1) * 512], mt == 0, mt == 1, [PT_, V])
            P.op(ACT, lambda e: e.activation(ot_[:, :], po[:, 0:512], AF.Copy, scale=st_[:, 3:4]),
                 reads=[po, st_], writes=[ot_])

        def stC(ui_):
            tt, hd = units[ui_]
            ot_ = otok[ui_ % 3]
            ts = slice(tt * 128, (tt + 1) * 128)
            pt2 = npb()
            for i in range(4):
                tp(pt2, pt2[:, i * 128:(i + 1) * 128], ot_[:, i * 128:(i + 1) * 128], ident_b(), [ot_, cbf])
            for i in range(4):
                j = 4 * hd + i
                if hd % 2:
                    P.op(ACT, lambda e, i=i, j=j: e.activation(oT[j][:, ts], pt2[:, i * 128:(i + 1) * 128], AF.Copy), reads=[pt2], writes=[oT[j]])
                else:
                    P.op(DVE, lambda e, i=i, j=j: e.tensor_copy(oT[j][:, ts], pt2[:, i * 128:(i + 1) * 128]), reads=[pt2], writes=[oT[j]])

        NU = len(units)
        for it in range(NU + 5):
            if it < NU:
                stA(it)
            if 0 <= it - 2 < NU:
                stB(it - 2)
            if 0 <= it - 3 < NU:
                stB2(it - 3)
            if 0 <= it - 5 < NU:
                stC(it - 5)
        Prog.cur_tag = "xa.wo"
        gemm_fm(w["wo"], l * D, KC, 0, D, lambda k: oT[k][:, :], oT, T, resid_add(1.0))
        P.release(m)

    P.dma(SP, prm, [(prm[:, :], prm_d[:, :])])
    P.dma(SP, cst, [(cst[:, :], cst_d[:, :])])
    P.op(DVE, lambda e: e.tensor_copy(cbf[:, :], cst[:, 0:384]), reads=[cst], writes=[cbf])
    oc = [0]
    for seq in range(NSEQ):
        for hp in range(NPASS):
            first = (hp == 0)
            tok0 = seq * S + hp * T
            passno[0] = seq * NPASS + hp
            blk[0] = 0
            if passno[0] == 1:
                wi = Ins(POOL, None)
                wi.deps = [Ev(t_.dsem, None, P.dsems[t_.dsem][1]) for t_ in wst if t_.dsem is not None]
                P.q[POOL].append(wi)
            Prog.cur_tag = "io.in"
            m = P.mark()
            stg = [P.sb("stg%d" % i, [128, D], F32) for i in range(2)]
            for tt in range(NTT):
                st_ = stg[tt % 2]
                r0 = tok0 + tt * 128
                P.dma(SP, st_, [(st_[:, :], x_d[r0:r0 + 128, :])])
                for k0 in range(0, KC, 4):
                    ps = nps()
                    for i in range(4):
                        tp(ps, ps[:, i * 128:(i + 1) * 128], st_[:, (k0 + i) * 128:(k0 + i + 1) * 128], ident_f(), [st_, cst])
                    for i in range(4):
                        k = k0 + i
                        if (k0 // 4) % 2:
                            P.op(ACT, lambda e, ps=ps, i=i, k=k, tt=tt: e.activation(h[k][:, tt * 128:(tt + 1) * 128], ps[:, i * 128:(i + 1) * 128], AF.Copy),
                                 reads=[ps], writes=[h[k]])
                        else:
                            P.op(DVE, lambda e, ps=ps, i=i, k=k, tt=tt: e.tensor_copy(h[k][:, tt * 128:(tt + 1) * 128], ps[:, i * 128:(i + 1) * 128]),
                                 reads=[ps], writes=[h[k]])
            P.release(m)
            for l in range(L):
                ffn(l, "ffn1", l * PL + 0)
                mixer(l, first, tok0)
                xattn(l, seq, first)
                ffn(l, "ffn2", l * PL + 64)
            Prog.cur_tag = "io.out"
            rmsnorm(L * PL, None, inplace=True)
            m = P.mark()
            ost = [P.sb("ost%d" % i, [128, D], F32) for i in range(2)]
            for tt in range(NTT):
                o_ = ost[tt % 2]
                for k0 in range(0, KC, 4):
                    ps = nps()
                    for i in range(4):
                        tp(ps, ps[:, i * 128:(i + 1) * 128], h[k0 + i][:, tt * 128:(tt + 1) * 128], ident_f(), [h[k0 + i], cst])
                    eng = ev_eng()
                    if eng == ACT:
                        P.op(ACT, lambda e, ps=ps, o_=o_, k0=k0: e.activation(o_[:, k0 * 128:(k0 + 4) * 128], ps[:, 0:512], AF.Copy), reads=[ps], writes=[o_])
                    else:
                        P.op(DVE, lambda e, ps=ps, o_=o_, k0=k0: e.tensor_copy(o_[:, k0 * 128:(k0 + 4) * 128], ps[:, 0:512]), reads=[ps], writes=[o_])
                r0 = tok0 + tt * 128
                od = out_tl[oc[0] % 2]
                oc[0] += 1
                P.dma(SP, od, [(out_d[r0:r0 + 128, :], o_[:, :])], src=o_)
            P.release(m)
    P.barrier(engs=(PE, ACT, DVE, POOL, SP), final=True)
    P.emit()
    return nc, P


def _consts():
    c = np.zeros((128, 448), np.float32)
    c[:, 0:128] = np.eye(128, dtype=np.float32)
    c[:, 128:256] = np.triu(np.ones((128, 128), np.float32))
    c[:, 256:384] = 1.0
    for g, wsz in enumerate((2, 4, 8, 16)):
        t = np.arange(16)
        c[:, 384 + g * 16:384 + (g + 1) * 16] = (1.0 / np.minimum(t + 1, wsz)).astype(np.float32)[None, :]
    return c


def _params(inp, L):
    prm = np.zeros((128, L * PL + 16), np.float32)

    def fm(v):
        return np.ascontiguousarray(np.asarray(v, np.float32).reshape(-1, 128).T)

    for l in range(L):
        b = l * PL
        prm[:, b + 0:b + 16] = fm(inp["ffn1_norm"][l])
        prm[:, b + 16:b + 32] = fm(inp["mix_norm"][l])
        prm[:, b + 32:b + 48] = fm(inp["xattn_norm"][l])
        prm[:, b + 48:b + 64] = fm(inp["mem_norm"][l])
        prm[:, b + 64:b + 80] = fm(inp["ffn2_norm"][l])
        cw = np.asarray(inp["qk_conv"][l], np.float32)
        prm[:, b + 80:b + 144] = cw.reshape(4, 16, 128).transpose(2, 1, 0).reshape(128, 64)
        prm[:, b + 144:b + 152] = fm(inp["pool_scale"][l])
        prm[:, b + 152:b + 160] = fm(inp["head_norm"][l])
        prm[:, b + 160:b + 168] = np.asarray(inp["gate_bias"][l], np.float32)[None, :]
    prm[:, L * PL:L * PL + 16] = fm(inp["final_norm"])
    return prm


_CACHE = {}


def run(inp, NSEQ, S, T, DEPTH, ncores, trace=False):
    key = (NSEQ, S, T, DEPTH)
    if key not in _CACHE:
        _CACHE[key] = build(NSEQ, S, T, DEPTH)
    nc, P = _CACHE[key]
    L = DEPTH
    f32 = lambda a: np.ascontiguousarray(np.asarray(a, np.float32))
    shared = {
        "prm": _params(inp, L), "cst": _consts(),
        "w_in": f32(inp["w_in"]).reshape(L * D, DIN), "w_out": f32(inp["w_out"]).reshape(L * D, D),
        "pool_w": f32(inp["pool_w"]).reshape(L * 1024, 256),
        "xattn_wq": f32(inp["xattn_wq"]).reshape(L * D, D), "xattn_wkv": f32(inp["xattn_wkv"]).reshape(L * D, 2 * D),
        "xattn_wo": f32(inp["xattn_wo"]).reshape(L * D, D),
    }
    for f in ("ffn1", "ffn2"):
        shared[f + "_w_gate"] = f32(inp[f + "_w_gate"]).reshape(L * D, FF)
        shared[f + "_w_up"] = f32(inp[f + "_w_up"]).reshape(L * D, FF)
        shared[f + "_w_down"] = f32(inp[f + "_w_down"]).reshape(L * FF, D)
    x = f32(inp["x"])
    mem = f32(inp["mem"])
    in_maps = []
    for c in range(ncores):
        d = dict(shared)
        d["x"] = x[c * NSEQ:(c + 1) * NSEQ].reshape(NSEQ * S, D)
        d["mem"] = mem[c * NSEQ:(c + 1) * NSEQ].reshape(NSEQ * NMEM, D)
        in_maps.append(d)
    res = run_bass_kernel_spmd(nc, in_maps, core_ids=list(range(ncores)), **({"trace": True} if trace else {}))
    out = np.stack([res.results[c]["out"].reshape(NSEQ, S, D) for c in range(ncores)], 0).reshape(ncores * NSEQ, S, D)
    return out.astype(np.float32), res


def kernel(**inputs):
    out, _ = run(inputs, NSEQ=2, S=2048, T=512, DEPTH=4, ncores=8)
    return out
```
